# Optimizing a Trainium2 kernel written in Bass

```python
import math
import jax, jax.numpy as jnp
from jax import lax
import numpy as np

D_MODEL = 2048
BATCH = 16
SEQ = 2048
DEPTH = 1

ROPE_THETA = 500000.0
EPS = 1e-6
Q_BLOCK = 128
MLA_HEADS = 8
MLA_Q_LORA = 512
MLA_KV_LORA = 512
MLA_NOPE = 128
MLA_ROPE = 64
MLA_V = 128
MLA_QK = MLA_NOPE + MLA_ROPE
DIFF_HEADS = 8
DIFF_D = 64
DIFF_V = 2 * DIFF_D
DIFF_ROT = DIFF_D // 4
COL_Q_LAT = MLA_Q_LORA
COL_KV_LAT = MLA_KV_LORA
COL_K_ROPE = MLA_ROPE
COL_DQ = DIFF_HEADS * 2 * DIFF_D
COL_DK = DIFF_HEADS * 2 * DIFF_D
COL_DV = DIFF_HEADS * DIFF_V
COL_GATES = 2 * D_MODEL
ATT_IN = COL_Q_LAT + COL_KV_LAT + COL_K_ROPE + COL_DQ + COL_DK + COL_DV + COL_GATES
PEER_HEADS = 8
PEER_NKEYS = 128
PEER_EXPERTS = PEER_NKEYS * PEER_NKEYS
PEER_HALF = 128
PEER_QDIM = 2 * PEER_HALF
PEER_TOPK = 16
PEER_CHUNK = 128

kernel_name = "hybrid_mla_diffattn_peer_block"


def rmsnorm(x, w):
    x32 = x.astype(jnp.float32)
    y = x32 * lax.rsqrt(jnp.mean(x32 * x32, axis=-1, keepdims=True) + EPS)
    return (y * w.astype(jnp.float32)).astype(x.dtype)


def rope(x, pos):
    r = x.shape[-1]
    inv = ROPE_THETA ** (-jnp.arange(0, r, 2, dtype=jnp.float32) / r)
    ang = pos.astype(jnp.float32)[..., None] * inv
    cos = jnp.cos(ang)[:, :, None, :]
    sin = jnp.sin(ang)[:, :, None, :]
    x32 = x.astype(jnp.float32)
    x1, x2 = x32[..., : r // 2], x32[..., r // 2 :]
    return jnp.concatenate([x1 * cos - x2 * sin, x1 * sin + x2 * cos], axis=-1).astype(x.dtype)


def blocked_causal_attention(q, k, v, scale, combine):
    B, S = q.shape[0], q.shape[1]
    nb = S // Q_BLOCK
    q_blocks = q.reshape(B, nb, Q_BLOCK, q.shape[2], q.shape[3]).swapaxes(0, 1)
    key_idx = jnp.arange(S)

    def one_block(args):
        qb, start = args
        s = jnp.einsum('bqhd,bkhd->bhqk', qb, k).astype(jnp.float32) * scale
        q_idx = start + jnp.arange(Q_BLOCK)
        s = jnp.where(key_idx[None, :] <= q_idx[:, None], s, jnp.finfo(jnp.float32).min)
        p = combine(jax.nn.softmax(s, axis=-1))
        return jnp.einsum('bhqk,bkhd->bqhd', p.astype(v.dtype), v)

    out = lax.map(one_block, (q_blocks, jnp.arange(nb) * Q_BLOCK))
    return out.swapaxes(0, 1).reshape(B, S, out.shape[3], out.shape[4])


def setup_inputs(seed: int = 0) -> dict:
    key = jax.random.key(seed)
    ks = iter(jax.random.split(key, 32))
    f32 = jnp.float32

    def nrm(shape, scale):
        return jax.random.normal(next(ks), shape, f32) * scale

    def gain(shape):
        return 1.0 + 0.02 * jax.random.normal(next(ks), shape, f32)

    L = DEPTH
    x = jax.random.normal(next(ks), (BATCH, SEQ, D_MODEL), f32)
    c = jax.random.normal(next(ks), (BATCH, D_MODEL), f32)
    offs = jax.random.randint(next(ks), (BATCH, 1), 0, 1024, dtype=jnp.int32)
    positions = jnp.arange(SEQ, dtype=jnp.int32)[None, :] + offs
    return {
        "x": x,
        "c": c,
        "positions": positions,
        "norm1_w": gain((L, D_MODEL)),
        "norm2_w": gain((L, D_MODEL)),
        "w_ada": nrm((L, D_MODEL, 6 * D_MODEL), 0.5 * D_MODEL ** -0.5),
        "b_ada": nrm((L, 6 * D_MODEL), 0.01),
        "w_att_in": nrm((L, D_MODEL, ATT_IN), D_MODEL ** -0.5),
        "mla_q_norm": gain((L, MLA_Q_LORA)),
        "w_mla_qb": nrm((L, MLA_Q_LORA, MLA_HEADS * MLA_QK), MLA_Q_LORA ** -0.5),
        "mla_kv_norm": gain((L, MLA_KV_LORA)),
        "w_mla_kvb": nrm((L, MLA_KV_LORA, MLA_HEADS * (MLA_NOPE + MLA_V)), MLA_KV_LORA ** -0.5),
        "mla_qk_norm_q": gain((L, MLA_QK)),
        "mla_qk_norm_k": gain((L, MLA_QK)),
        "w_mla_o": nrm((L, MLA_HEADS * MLA_V, D_MODEL), (MLA_HEADS * MLA_V) ** -0.5),
        "diff_q_norm": gain((L, DIFF_D)),
        "diff_k_norm": gain((L, DIFF_D)),
        "diff_lambda": nrm((L, 4, DIFF_D), 0.1),
        "diff_subln": gain((L, DIFF_V)),
        "w_diff_o": nrm((L, DIFF_HEADS * DIFF_V, D_MODEL), (DIFF_HEADS * DIFF_V) ** -0.5),
        "w_att_out": nrm((L, D_MODEL, D_MODEL), D_MODEL ** -0.5),
        "w_peer_q": nrm((L, D_MODEL, PEER_HEADS * PEER_QDIM), D_MODEL ** -0.5),
        "peer_keys": nrm((L, PEER_HEADS, 2, PEER_NKEYS, PEER_HALF), PEER_HALF ** -0.5),
        "peer_u": nrm((L, PEER_EXPERTS, D_MODEL), D_MODEL ** -0.5),
        "peer_v": nrm((L, PEER_EXPERTS, D_MODEL), 0.5),
    }


def reference(x, c, positions, norm1_w, norm2_w, w_ada, b_ada, w_att_in, mla_q_norm, w_mla_qb,
              mla_kv_norm, w_mla_kvb, mla_qk_norm_q, mla_qk_norm_k, w_mla_o, diff_q_norm,
              diff_k_norm, diff_lambda, diff_subln, w_diff_o, w_att_out, w_peer_q, peer_keys,
              peer_u, peer_v):
    B, S, D = x.shape
    for l in range(DEPTH):
        lambda_init = 0.8 - 0.6 * math.exp(-0.3 * l)
        mod = jax.nn.silu(c) @ w_ada[l] + b_ada[l]
        sh1, sc1, g1, sh2, sc2, g2 = jnp.split(mod, 6, axis=-1)

        h = rmsnorm(x, norm1_w[l]) * (1.0 + sc1[:, None, :]) + sh1[:, None, :]
        proj = h @ w_att_in[l]
        splits = np.cumsum([COL_Q_LAT, COL_KV_LAT, COL_K_ROPE, COL_DQ, COL_DK, COL_DV, D_MODEL])
        q_lat, kv_lat, k_rope, dq, dk, dv, gate_a, gate_b = jnp.split(proj, splits, axis=-1)

        q = (rmsnorm(q_lat, mla_q_norm[l]) @ w_mla_qb[l]).reshape(B, S, MLA_HEADS, MLA_QK)
        kv = (rmsnorm(kv_lat, mla_kv_norm[l]) @ w_mla_kvb[l]).reshape(B, S, MLA_HEADS, MLA_NOPE + MLA_V)
        k_nope, v_a = kv[..., :MLA_NOPE], kv[..., MLA_NOPE:]
        k_r = jnp.broadcast_to(k_rope[:, :, None, :], (B, S, MLA_HEADS, MLA_ROPE))
        k = jnp.concatenate([k_nope, k_r], axis=-1)
        q = rmsnorm(q, mla_qk_norm_q[l])
        k = rmsnorm(k, mla_qk_norm_k[l])
        q = jnp.concatenate([q[..., :MLA_NOPE], rope(q[..., MLA_NOPE:], positions)], axis=-1)
        k = jnp.concatenate([k[..., :MLA_NOPE], rope(k[..., MLA_NOPE:], positions)], axis=-1)
        o_a = blocked_causal_attention(q, k, v_a, MLA_QK ** -0.5, lambda p: p)
        y_a = o_a.reshape(B, S, MLA_HEADS * MLA_V) @ w_mla_o[l]

        dq = rmsnorm(dq.reshape(B, S, 2 * DIFF_HEADS, DIFF_D), diff_q_norm[l])
        dk = rmsnorm(dk.reshape(B, S, 2 * DIFF_HEADS, DIFF_D), diff_k_norm[l])
        dq = jnp.concatenate([rope(dq[..., :DIFF_ROT], positions), dq[..., DIFF_ROT:]], axis=-1)
        dk = jnp.concatenate([rope(dk[..., :DIFF_ROT], positions), dk[..., DIFF_ROT:]], axis=-1)
        v_b = dv.reshape(B, S, DIFF_HEADS, DIFF_V)
        lam_p = diff_lambda[l].astype(jnp.float32)
        lam = (jnp.exp(jnp.sum(lam_p[0] * lam_p[1])) - jnp.exp(jnp.sum(lam_p[2] * lam_p[3]))
               + lambda_init)

        def diff_combine(p):
            p = p.reshape(p.shape[0], DIFF_HEADS, 2, p.shape[2], p.shape[3])
            return p[:, :, 0] - lam * p[:, :, 1]

        o_b = blocked_causal_attention(dq, dk, v_b, DIFF_D ** -0.5, diff_combine)
        o_b = rmsnorm(o_b, diff_subln[l]) * (1.0 - lambda_init)
        y_b = o_b.reshape(B, S, DIFF_HEADS * DIFF_V) @ w_diff_o[l]

        merged = jax.nn.sigmoid(gate_a) * y_a + jax.nn.sigmoid(gate_b) * y_b
        x = x + g1[:, None, :] * (merged @ w_att_out[l])

        h = rmsnorm(x, norm2_w[l]) * (1.0 + sc2[:, None, :]) + sh2[:, None, :]
        pq = (h @ w_peer_q[l]).reshape(B, S, PEER_HEADS, 2, PEER_HALF)
        sub = jnp.einsum('bshpd,hpnd->bshpn', pq, peer_keys[l]).astype(jnp.float32)
        sv, si = lax.top_k(sub, PEER_TOPK)
        cand = (sv[..., 0, :, None] + sv[..., 1, None, :]).reshape(B, S, PEER_HEADS, PEER_TOPK * PEER_TOPK)
        cidx = (si[..., 0, :, None] * PEER_NKEYS + si[..., 1, None, :]).reshape(B, S, PEER_HEADS, PEER_TOPK * PEER_TOPK)
        top, pos = lax.top_k(cand, PEER_TOPK)
        eidx = jnp.take_along_axis(cidx, pos, axis=-1)
        gates = jax.nn.softmax(top, axis=-1)
        T = B * S
        nch = T // PEER_CHUNK
        h_c = h.reshape(nch, PEER_CHUNK, D)
        i_c = eidx.reshape(nch, PEER_CHUNK, PEER_HEADS * PEER_TOPK)
        g_c = gates.reshape(nch, PEER_CHUNK, PEER_HEADS * PEER_TOPK)
        u_tab, v_tab = peer_u[l], peer_v[l]

        def peer_chunk(args):
            hc, ic, gc = args
            u = jnp.take(u_tab, ic, axis=0)
            a = jax.nn.gelu(jnp.einsum('cd,ckd->ck', hc, u).astype(jnp.float32), approximate=False)
            w = jnp.take(v_tab, ic, axis=0)
            return jnp.einsum('ck,ckd->cd', (gc * a).astype(hc.dtype), w)

        ffn = lax.map(peer_chunk, (h_c, i_c, g_c)).reshape(B, S, D)
        x = x + g2[:, None, :] * ffn
    return x
```

```python
import os, math
from contextlib import ExitStack
import numpy as np
import concourse.bass as bass
import concourse.mybir as mybir
from concourse.bass_utils import run_bass_kernel_spmd

F32 = mybir.dt.float32
BF16 = mybir.dt.bfloat16
U32 = mybir.dt.uint32
I32 = mybir.dt.int32
AF = mybir.ActivationFunctionType
ALU = mybir.AluOpType
AX = mybir.AxisListType

NB = 2
S = 2048
D = 2048
NT = S // 128
ATT_IN = 8256
EPS = 1e-6
LAMBDA_INIT = 0.8 - 0.6 * math.exp(-0.3 * 0)
C_GA = 4160
C_GB = 6208
NEG = -1.0e30


class Sem:
    def __init__(self, nc, name, dma=False):
        self.h = nc.semaphore(name).__enter__()
        self.name = name
        self.cnt = 0
        self.dma = dma


class Tk:
    __slots__ = ("sem", "val")

    def __init__(self, sem, val):
        self.sem = sem
        self.val = val


class Buf:
    def __init__(self, name):
        self.name = name
        self.w = {}
        self.r = {}


class Eng:
    def __init__(self, nc, e, name):
        self.e = e
        self.name = name
        self.sem = Sem(nc, "s_" + name)
        self.waited = {}
        self.is_pe = name == "pe"

    def wait_for(self, tk):
        val = tk.sem.cnt if tk.sem.dma else tk.val
        if self.waited.get(tk.sem.name, 0) >= val:
            return
        self.e.wait_ge(tk.sem.h, val)
        self.waited[tk.sem.name] = val


class Tile:
    def __init__(self, K, h, name):
        self.h = h
        self.ap = h.ap()
        self.buf = Buf(name)
        self.K = K
        self._ds = None

    def __getitem__(self, k):
        return self.ap[k]

    @property
    def ds(self):
        if self._ds is None:
            self._ds = self.K.next_ds()
        return self._ds


class KB:
    def __init__(self, nc):
        self.nc = nc
        self.PE = Eng(nc, nc.tensor, "pe")
        self.ACT = Eng(nc, nc.scalar, "act")
        self.DVE = Eng(nc, nc.vector, "dve")
        self.POOL = Eng(nc, nc.gpsimd, "pool")
        self.SP = Eng(nc, nc.sync, "sp")
        self.engs = [self.PE, self.ACT, self.DVE, self.POOL, self.SP]
        self.dsems = [Sem(nc, "d%d" % i, dma=True) for i in range(56)]
        self.ds_i = 0

    def next_ds(self):
        s = self.dsems[self.ds_i % len(self.dsems)]
        self.ds_i += 1
        return s

    def sb(self, es, name, shape, dt):
        self.nm = getattr(self, "nm", 0) + 1
        name = "t%d_%s" % (self.nm, name)
        h = es.enter_context(self.nc.sbuf_tensor(name, list(shape), dt))
        return Tile(self, h, name)

    def _deps(self, E, r, w):
        for b in r:
            b = getattr(b, "buf", b)
            for tk in b.w.values():
                if not (E.is_pe and tk.sem is E.sem):
                    E.wait_for(tk)
        for b in w:
            b = getattr(b, "buf", b)
            for tk in list(b.w.values()) + list(b.r.values()):
                if not (E.is_pe and tk.sem is E.sem):
                    E.wait_for(tk)

    def _upd(self, tk, r, w, wa=()):
        for b in r:
            b = getattr(b, "buf", b)
            b.r[tk.sem.name] = tk
        for b in w:
            b = getattr(b, "buf", b)
            b.w = {tk.sem.name: tk}
            b.r = {}
        for b in wa:
            b = getattr(b, "buf", b)
            b.w[tk.sem.name] = tk

    def op(self, E, fn, r=(), w=(), inc=True):
        self._deps(E, r, w)
        inst = fn()
        if inc:
            E.sem.cnt += 1
            inst.then_inc(E.sem.h, 1)
            tk = Tk(E.sem, E.sem.cnt)
        else:
            tk = Tk(E.sem, E.sem.cnt + 1)
        self._upd(tk, r, w)
        return tk

    def dma(self, Q, out, in_, ds, r=(), w=(), wa=(), **kw):
        self._deps(Q, r, list(w) + list(wa))
        Q.e.dma_start(out=out, in_=in_, **kw).then_inc(ds.h, 16)
        ds.cnt += 16
        tk = Tk(ds, ds.cnt)
        self._upd(tk, r, w, wa)
        return tk

    def dump(self, name, t, dt, shape):
        if not self.dbg:
            return
        d = self.nc.dram_tensor("dbg_" + name, list(shape), dt, kind="ExternalOutput").ap()
        self.dma(self.SP, d, t.ap, t.ds, r=[t])

    def barrier(self):
        sems = [e.sem for e in self.engs] + self.dsems
        for E in self.engs:
            for s in sems:
                if s.cnt > 0 and E.waited.get(s.name, 0) < s.cnt:
                    E.e.wait_ge(s.h, s.cnt)
                    E.waited[s.name] = s.cnt


def build_nc(stage=99, dbg=False):
    nc = bass.Bass("TRN2", target_bir_lowering=False)
    K = KB(nc)
    K.dbg = dbg
    PE, ACT, DVE, POOL, SP = K.PE, K.ACT, K.DVE, K.POOL, K.SP
    pe, act, dve, pool = nc.tensor, nc.scalar, nc.vector, nc.gpsimd
    STQ = SP if os.environ.get("KB_STQ", "sp") == "sp" else POOL

    def din(name, shape, dt=F32):
        return nc.dram_tensor(name, list(shape), dt, kind="ExternalInput").ap()

    def dscr(name, shape, dt=F32):
        kind = "ExternalOutput" if (dbg and name in DBG_OUT) else "Internal"
        return nc.dram_tensor(name, list(shape), dt, kind=kind).ap()

    x_d = din("x", [NB, S, D])
    cT_d = din("cT", [128, 16, NB])
    pos_d = din("posT", [128, NB, NT], I32)
    n1w_d = din("n1wT", [128, 16])
    n2w_d = din("n2wT", [128, 16])
    wada_d = din("w_ada", [D, 6 * D])
    bada_d = din("b_adaT", [128, 96])
    watt_d = din("w_att_in", [D, ATT_IN])
    gq_lat_d = din("mla_q_norm_bc", [128, 512])
    gkv_lat_d = din("mla_kv_norm_bc", [128, 512])
    wqb_d = din("w_mla_qb", [512, 1536])
    wkvb_d = din("w_mla_kvb", [512, 2048])
    gq_d = din("mla_qk_norm_q_bc", [128, 192])
    gk_d = din("mla_qk_norm_k_bc", [128, 192])
    wmo_d = din("w_mla_o", [1024, D])
    gdq_d = din("diff_q_norm_bc", [128, 64])
    gdk_d = din("diff_k_norm_bc", [128, 64])
    dlam_d = din("diff_lambda_bc", [128, 4, 64])
    gsub_d = din("diff_subln_bc", [128, 128])
    wdo_d = din("w_diff_o", [1024, D])
    wao_d = din("w_att_out", [D, D])
    wpq_d = din("w_peer_q", [D, D])
    keysT_d = din("peer_keysT", [128, 16, 128])
    uT_d = din("peer_uT", [128, 128, 2048])
    v_d = din("peer_v", [128, 128, 2048])
    iota_d = din("iota128", [128, 128])
    inv_mla_d = din("inv_mla", [128, 32])
    inv_dif_d = din("inv_dif", [128, 8])
    out_d = nc.dram_tensor("out", [NB, S, D], F32, kind="ExternalOutput").ap()

    proj_d = [dscr("proj%d" % i, [S, ATT_IN]) for i in range(NB)]
    if os.environ.get("KB_SWAP"):
        proj_d = proj_d[::-1]
    qT_d = dscr("qT", [8, 192, S], BF16)
    kT_d = dscr("kT", [8, 192, S], BF16)
    va_d = dscr("va", [8, 128, NT, 130], BF16)
    dqT_d = dscr("dqT", [16, 64, S], BF16)
    dkT_d = dscr("dkT", [16, 64, S], BF16)
    vb_d = dscr("vb", [8, 128, NT, 130], BF16)
    h2T_d = dscr("h2T", [128, 16, S], BF16)
    sub_d = dscr("sub", [S, 2048])
    ub_d = dscr("ub", [128, 128, 2048], BF16)
    vbf_d = dscr("vbf", [128, 128, 2048], BF16)
    B_proj = [Buf("proj0"), Buf("proj1")]
    B_qk = Buf("qkscr")
    B_dqk = Buf("dqkscr")
    B_h2T = Buf("h2Tscr")
    B_sub = Buf("subscr")
    B_tab = Buf("tabscr")
    B_out = [Buf("out0"), Buf("out1")]

    psb = []
    for i in range(8):
        h = nc.alloc_psum_tensor("psb%d" % i, [128, 512], F32)
        t = Tile(K, h, "psb%d" % i)
        psb.append(t)
    ps_mm = psb[0:2]
    ps_tr = psb[2:4]
    ps_acc = psb[4:8]

    def trv(t):
        return t.ap.bitcast(BF16).rearrange("p (a b) -> p a b", a=8)

    top = ExitStack()
    ident = K.sb(top, "ident", [128, 128], BF16)
    identf = K.sb(top, "identf", [128, 128], F32)
    tri = K.sb(top, "tri", [128, 128], BF16)
    modT = K.sb(top, "modT", [128, 96, NB], F32)
    G1T = K.sb(top, "G1T", [128, 16, NB], F32)
    G2T = K.sb(top, "G2T", [128, 16, NB], F32)
    neglam = K.sb(top, "neglam", [128, 1], F32)
    iota = K.sb(top, "iota", [128, 128], F32)
    posf = K.sb(top, "posf", [128, NB, NT], F32)
    inv_mla = K.sb(top, "inv_mla", [128, 32], F32)
    inv_dif = K.sb(top, "inv_dif", [128, 8], F32)

    modP = [K.sb(top, "modP%d" % i, [128, 96, 2], F32) for i in range(NB)]
    GP1 = [K.sb(top, "GP1%d" % i, [128, 16, 2], F32) for i in range(NB)]
    GP2 = [K.sb(top, "GP2%d" % i, [128, 16, 2], F32) for i in range(NB)]
    epsc = K.sb(top, "epsc", [128, 1], F32)
    mhalf = K.sb(top, "mhalf", [128, 1], F32)
    K.op(POOL, lambda: pool.memset(mhalf.ap, -0.5), w=[mhalf])
    K.op(POOL, lambda: pool.memset(epsc.ap, EPS), w=[epsc])
    for t_ in (ident, identf):
        K.op(POOL, lambda: pool.memset(t_.ap, 1.0), w=[t_])
        K.op(POOL, lambda: pool.affine_select(out=t_.ap, in_=t_.ap, pattern=[[-1, 128]], compare_op=ALU.is_equal,
                                              fill=0.0, base=0, channel_multiplier=1), r=[t_], w=[t_])
    K.op(POOL, lambda: pool.memset(tri.ap, 1.0), w=[tri])
    K.op(POOL, lambda: pool.affine_select(out=tri.ap, in_=tri.ap, pattern=[[1, 128]], compare_op=ALU.is_ge,
                                          fill=0.0, base=0, channel_multiplier=-1), r=[tri], w=[tri])
    K.dma(SP, iota.ap, iota_d, iota.ds, w=[iota])
    K.dma(SP, inv_mla.ap, inv_mla_d, inv_mla.ds, w=[inv_mla])
    K.dma(SP, inv_dif.ap, inv_dif_d, inv_dif.ds, w=[inv_dif])
    posi = K.sb(top, "posi", [128, NB, NT], I32)
    K.dma(SP, posi.ap, pos_d, posi.ds, w=[posi])
    K.op(DVE, lambda: dve.tensor_copy(out=posf.ap, in_=posi.ap), r=[posi], w=[posf])

    with ExitStack() as es:
        cT = K.sb(es, "cT", [128, 16, NB], F32)
        scT = K.sb(es, "scT", [128, 16, NB], F32)
        bada = K.sb(es, "bada", [128, 96], F32)
        n1w = K.sb(es, "n1w", [128, 16], F32)
        n2w = K.sb(es, "n2w", [128, 16], F32)
        dl = K.sb(es, "dl", [128, 4, 64], F32)
        dl2 = K.sb(es, "dl2", [128, 2, 64], F32)
        ls = K.sb(es, "ls", [128, 2], F32)
        wts = [K.sb(es, "wada%d" % i, [128, 16, 512], F32) for i in range(2)]
        K.dma(SP, cT.ap, cT_d, cT.ds, w=[cT])
        K.dma(SP, bada.ap, bada_d, bada.ds, w=[bada])
        K.dma(SP, n1w.ap, n1w_d, n1w.ds, w=[n1w])
        K.dma(SP, n2w.ap, n2w_d, n2w.ds, w=[n2w])
        K.dma(SP, dl.ap, dlam_d, dl.ds, w=[dl])
        K.op(ACT, lambda: act.activation(out=scT.ap, in_=cT.ap, func=AF.Silu), r=[cT], w=[scT])
        K.op(DVE, lambda: dve.tensor_tensor(out=dl2.ap, in0=dl.ap.rearrange("p (a b) d -> p a b d", b=2)[:, :, 0, :],
                                            in1=dl.ap.rearrange("p (a b) d -> p a b d", b=2)[:, :, 1, :], op=ALU.mult),
             r=[dl], w=[dl2])
        K.op(DVE, lambda: dve.tensor_reduce(out=ls.ap, in_=dl2.ap, axis=AX.X, op=ALU.add), r=[dl2], w=[ls])
        K.op(ACT, lambda: act.activation(out=ls.ap, in_=ls.ap, func=AF.Exp), r=[ls], w=[ls])
        K.op(DVE, lambda: dve.tensor_tensor(out=neglam.ap, in0=ls[:, 1:2], in1=ls[:, 0:1], op=ALU.subtract),
             r=[ls], w=[neglam])
        K.op(DVE, lambda: dve.tensor_scalar(out=neglam.ap, in0=neglam.ap, scalar1=-LAMBDA_INIT, scalar2=None,
                                            op0=ALU.add), r=[neglam], w=[neglam])
        conv = []
        if stage >= 10:
            tf = [K.sb(es, "tf%d" % i, [128, 2048], F32) for i in range(4)]
            tb = [K.sb(es, "tb%d" % i, [128, 2048], BF16) for i in range(4)]
            n_ = 0
            for (src_, dst_) in ((uT_d, ub_d), (v_d, vbf_d)):
                for i_ in range(128):
                    def cv(f=tf[n_ % 4], bb=tb[n_ % 4], sa=src_[i_], da=dst_[i_], par=n_ % 2):
                        K.dma(SP, f.ap, sa, f.ds, w=[f])
                        if par == 0:
                            K.op(ACT, lambda: act.copy(out=bb.ap, in_=f.ap), r=[f], w=[bb])
                        else:
                            K.op(DVE, lambda: dve.tensor_copy(out=bb.ap, in_=f.ap), r=[f], w=[bb])
                        K.dma(ACT, da, bb.ap, bb.ds, r=[bb], wa=[B_tab])
                    conv.append(cv)
                    n_ += 1
        wv = wada_d.rearrange("(kc p) n -> p kc n", p=128)
        pm = psb[0]
        for blk in range(24):
            for _ in range(min(11, len(conv))):
                conv.pop(0)()
            wt = wts[blk % 2]
            K.dma(SP, wt.ap, wv[:, :, blk * 512:(blk + 1) * 512], wt.ds, w=[wt])
            for oc in range(4):
                col = (blk * 4 + oc) * NB
                for kc in range(16):
                    K.op(PE, lambda: pe.matmul(pm[:, col:col + NB], lhsT=wt[:, kc, oc * 128:(oc + 1) * 128],
                                               rhs=scT[:, kc, :], start=(kc == 0), stop=(kc == 15)),
                         r=[wt, scT], w=[pm], inc=(kc == 15))
        while conv:
            conv.pop(0)()
        K.op(DVE, lambda: dve.tensor_tensor(out=modT.ap, in0=pm[:, 0:96 * NB].rearrange("p (c b) -> p c b", b=NB),
                                            in1=bada.ap.unsqueeze(2).to_broadcast([128, 96, NB]), op=ALU.add),
             r=[pm, bada], w=[modT])
        for (GT, lo, nw) in ((G1T, 16, n1w), (G2T, 64, n2w)):
            K.op(DVE, lambda: dve.tensor_scalar(out=GT.ap, in0=modT[:, lo:lo + 16, :], scalar1=1.0, scalar2=None,
                                                op0=ALU.add), r=[modT], w=[GT])
            K.op(DVE, lambda: dve.tensor_tensor(out=GT.ap, in0=GT.ap, in1=nw.ap.unsqueeze(2).to_broadcast([128, 16, NB]),
                                                op=ALU.mult), r=[GT, nw], w=[GT])
    for i in range(NB):
        for (dst_, src_) in ((modP[i], modT), (GP1[i], G1T), (GP2[i], G2T)):
            K.op(DVE, lambda: dve.tensor_copy(out=dst_[:, :, 0:1], in_=src_[:, :, i:i + 1]), r=[src_], w=[dst_])
    K.dump("modT", modT, F32, [128, 96, NB])
    K.dump("G1T", G1T, F32, [128, 16, NB])
    K.dump("neglam", neglam, F32, [128, 1])
    K.barrier()
    SH1, G1, SH2, G2 = 0, 32, 48, 80

    def rms_stats(es_tiles, src_ap, n, ss_ap, junk_ap, rbufs, ss_t):
        K.op(ACT, lambda: act.activation(out=junk_ap, in_=src_ap, func=AF.Square, accum_out=ss_ap), r=rbufs,
             w=[ss_t] + es_tiles)
        K.op(ACT, lambda: act.activation(out=ss_ap, in_=ss_ap, func=AF.Sqrt, scale=1.0 / n, bias=epsc[:, 0:1]), r=[ss_t, epsc], w=[ss_t])
        K.op(DVE, lambda: dve.reciprocal(out=ss_ap, in_=ss_ap), r=[ss_t], w=[ss_t])

    evac_rr = [0]

    def evac_copy(out_ap, in_ap, r, w):
        evac_rr[0] += 1
        if evac_rr[0] % 2:
            return K.op(ACT, lambda: act.copy(out=out_ap, in_=in_ap), r=r, w=w)
        return K.op(DVE, lambda: dve.tensor_copy(out=out_ap, in_=in_ap), r=r, w=w)

    def load_w_block(wf, wb, w_dram, KC, c0, ncols, conv_eng):
        wv_ = w_dram.rearrange("(kc p) n -> p kc n", p=128)
        K.dma(SP, wf[:, 0:KC, 0:ncols], wv_[:, :, c0:c0 + ncols], wf.ds, w=[wf])
        if conv_eng is POOL:
            K.op(POOL, lambda: pool.tensor_copy(out=wb[:, 0:KC, 0:ncols], in_=wf[:, 0:KC, 0:ncols]), r=[wf], w=[wb])
        else:
            K.op(DVE, lambda: dve.tensor_copy(out=wb[:, 0:KC, 0:ncols], in_=wf[:, 0:KC, 0:ncols]), r=[wf], w=[wb])

    def norm_to_T(es, b, src_d, src_buf, GTsrc, shoff, hT, tagp):
        Gbc = K.sb(es, tagp + "Gbc", [128, D], F32)
        Sbc = K.sb(es, tagp + "Sbc", [128, D], F32)
        make_bc(Gbc, 0, b, GTsrc)
        make_bc(Sbc, shoff, b)
        xts = [K.sb(es, tagp + "xt%d" % i, [128, D], F32) for i in range(2)]
        xns = [K.sb(es, tagp + "xn%d" % i, [128, D], BF16) for i in range(2)]
        junk = K.sb(es, tagp + "junk", [128, D], BF16)
        sss = [K.sb(es, tagp + "ss%d" % i, [128, 1], F32) for i in range(2)]
        for tt in range(NT):
            xt, xn, ss = xts[tt % 2], xns[tt % 2], sss[tt % 2]
            K.dma(SP, xt.ap, src_d[b, tt * 128:(tt + 1) * 128, :], xt.ds, r=[src_buf], w=[xt])
            rms_stats([junk], xt.ap, D, ss.ap, junk.ap, [xt], ss)
            K.op(DVE, lambda: dve.scalar_tensor_tensor(out=xt.ap, in0=xt.ap, scalar=ss[:, 0:1], in1=Gbc.ap, op0=ALU.mult,
                                                       op1=ALU.mult), r=[xt, ss, Gbc], w=[xt])
            K.op(POOL, lambda: pool.tensor_tensor(out=xn.ap, in0=xt.ap, in1=Sbc.ap, op=ALU.add), r=[xt, Sbc], w=[xn])
            for c8 in range(2):
                pt = ps_tr[c8]
                for j in range(8):
                    ch = c8 * 8 + j
                    K.op(PE, lambda: pe.transpose(out=trv(pt)[:, j, :], in_=xn[:, ch * 128:(ch + 1) * 128],
                                                  identity=ident.ap), r=[xn, ident], w=[pt], inc=(j == 7))
                if c8 == 0:
                    K.op(ACT, lambda: act.copy(out=hT[:, 0:8, tt * 128:(tt + 1) * 128], in_=trv(pt)), r=[pt], w=[hT])
                else:
                    K.op(DVE, lambda: dve.tensor_copy(out=hT[:, 8:16, tt * 128:(tt + 1) * 128], in_=trv(pt)), r=[pt], w=[hT])

    def linear(es, aT, KC, w_dram, N, evac, tagp, conv_eng=POOL):
        wfs = [K.sb(es, tagp + "wf%d" % i, [128, KC, 512], F32) for i in range(2)]
        wbs = [K.sb(es, tagp + "wb%d" % i, [128, KC, 512], BF16) for i in range(2)]
        nblk = (N + 511) // 512
        cnt = 0
        load_w_block(wfs[0], wbs[0], w_dram, KC, 0, min(512, N), conv_eng)
        for nb in range(nblk):
            c0 = nb * 512
            ncols = min(512, N - c0)
            wf, wb = wfs[nb % 2], wbs[nb % 2]
            if nb + 1 < nblk:
                load_w_block(wfs[(nb + 1) % 2], wbs[(nb + 1) % 2], w_dram, KC, c0 + 512, min(512, N - c0 - 512), conv_eng)
            for tt in range(NT):
                ps = ps_mm[cnt % 2]
                cnt += 1
                for kc in range(KC):
                    K.op(PE, lambda: pe.matmul(ps[:, 0:ncols], lhsT=aT[:, kc, tt * 128:(tt + 1) * 128],
                                               rhs=wb[:, kc, 0:ncols], start=(kc == 0), stop=(kc == KC - 1)),
                         r=[aT, wb], w=[ps], inc=(kc == KC - 1))
                evac(ps, nb, c0, ncols, tt)

    def make_bc(dst, off, b, src=None):
        with ExitStack() as es2:
            tmp = K.sb(es2, "bctmp", [128, 16, 128], F32)
            src = modT if src is None else src
            K.op(DVE, lambda: dve.tensor_copy(out=tmp.ap, in_=src[:, off:off + 16, b:b + 1].to_broadcast([128, 16, 128])),
                 r=[src], w=[tmp])
            for q4 in range(4):
                ps = ps_mm[q4 % 2]
                for j in range(4):
                    ch = q4 * 4 + j
                    K.op(PE, lambda: pe.matmul(ps[:, j * 128:(j + 1) * 128], lhsT=tmp[:, ch, :], rhs=identf.ap,
                                               start=True, stop=True), r=[tmp, identf], w=[ps], inc=(j == 3))
                K.op(ACT, lambda: act.copy(out=dst[:, q4 * 512:(q4 + 1) * 512], in_=ps.ap), r=[ps], w=[dst])
            K.barrier()

    def rope_tables(es, b, inv, nf, tagp):
        ang = K.sb(es, tagp + "ang", [128, NT, nf], F32)
        cs = K.sb(es, tagp + "cos", [128, NT, nf], F32)
        sn = K.sb(es, tagp + "sin", [128, NT, nf], F32)
        tmp = K.sb(es, tagp + "rtmp", [128, NT, nf], F32)
        K.op(DVE, lambda: dve.tensor_tensor(out=ang.ap, in0=posf[:, b, :].unsqueeze(2).to_broadcast([128, NT, nf]),
                                            in1=inv.ap.unsqueeze(1).to_broadcast([128, NT, nf]), op=ALU.mult),
             r=[posf, inv], w=[ang])
        two_pi = 2.0 * math.pi
        ki = K.sb(es, tagp + "ki", [128, NT, nf], I32)
        kf = K.sb(es, tagp + "kf", [128, NT, nf], F32)
        for (shift, dst) in ((0.0, sn), (0.5 * math.pi, cs)):
            K.op(DVE, lambda: dve.tensor_scalar(out=tmp.ap, in0=ang.ap, scalar1=shift, scalar2=None, op0=ALU.add),
                 r=[ang], w=[tmp])
            K.op(DVE, lambda: dve.tensor_scalar(out=kf.ap, in0=tmp.ap, scalar1=1.0 / two_pi, scalar2=None, op0=ALU.mult),
                 r=[tmp], w=[kf])
            K.op(DVE, lambda: dve.tensor_copy(out=ki.ap, in_=kf.ap), r=[kf], w=[ki])
            K.op(DVE, lambda: dve.tensor_copy(out=kf.ap, in_=ki.ap), r=[ki], w=[kf])
            K.op(DVE, lambda: dve.scalar_tensor_tensor(out=tmp.ap, in0=kf.ap, scalar=-two_pi, in1=tmp.ap, op0=ALU.mult,
                                                       op1=ALU.add), r=[kf, tmp], w=[tmp])
            K.op(DVE, lambda: dve.tensor_scalar(out=kf.ap, in0=tmp.ap, scalar1=math.pi, scalar2=-two_pi, op0=ALU.is_gt,
                                                op1=ALU.mult), r=[tmp], w=[kf])
            K.op(DVE, lambda: dve.tensor_tensor(out=tmp.ap, in0=tmp.ap, in1=kf.ap, op=ALU.add), r=[tmp, kf], w=[tmp])
            K.op(DVE, lambda: dve.tensor_scalar(out=kf.ap, in0=tmp.ap, scalar1=-math.pi, scalar2=two_pi, op0=ALU.is_lt,
                                                op1=ALU.mult), r=[tmp], w=[kf])
            K.op(DVE, lambda: dve.tensor_tensor(out=tmp.ap, in0=tmp.ap, in1=kf.ap, op=ALU.add), r=[tmp, kf], w=[tmp])
            K.op(DVE, lambda: dve.tensor_scalar(out=tmp.ap, in0=tmp.ap, scalar1=3.1415925, scalar2=-3.1415925, op0=ALU.min,
                                                op1=ALU.max), r=[tmp], w=[tmp])
            K.op(ACT, lambda: act.activation(out=dst.ap, in_=tmp.ap, func=AF.Sin), r=[tmp], w=[dst])
        return cs, sn

    def hn_rope(src, H, d, gain, r0, hr, cs_ap, sn_ap, dst, T1, T2, R1, R2, SSq, rb):
        sv_ = src.ap[:, 0:H * d].rearrange("p (h d) -> p h d", h=H)
        t1 = T1.ap[:, 0:H * d].rearrange("p (h d) -> p h d", h=H)
        t2 = T2.ap[:, 0:H * d].rearrange("p (h d) -> p h d", h=H)
        dv_ = dst.ap[:, 0:H * d].rearrange("p (h d) -> p h d", h=H)
        ssq = SSq.ap[:, 0:H]
        K.op(POOL, lambda: pool.tensor_tensor(out=t1, in0=sv_, in1=sv_, op=ALU.mult), r=[src], w=[T1])
        K.op(DVE, lambda: dve.tensor_reduce(out=ssq, in_=t1, axis=AX.X, op=ALU.add), r=[T1], w=[SSq])
        K.op(ACT, lambda: act.activation(out=ssq, in_=ssq, func=AF.Sqrt, scale=1.0 / d, bias=epsc[:, 0:1]), r=[SSq, epsc], w=[SSq])
        K.op(DVE, lambda: dve.reciprocal(out=ssq, in_=ssq), r=[SSq], w=[SSq])
        K.op(DVE, lambda: dve.tensor_tensor(out=t1, in0=sv_, in1=ssq.unsqueeze(2).to_broadcast([128, H, d]), op=ALU.mult),
             r=[src, SSq], w=[T1])
        K.op(POOL, lambda: pool.tensor_tensor(out=t2, in0=t1, in1=gain.ap.unsqueeze(1).to_broadcast([128, H, d]),
                                              op=ALU.mult), r=[T1, gain], w=[T2])
        K.op(ACT, lambda: act.copy(out=dv_, in_=t2), r=[T2], w=[dst])
        x1 = t2[:, :, r0:r0 + hr]
        x2 = t2[:, :, r0 + hr:r0 + 2 * hr]
        cb = cs_ap.unsqueeze(1).to_broadcast([128, H, hr])
        sb_ = sn_ap.unsqueeze(1).to_broadcast([128, H, hr])
        ra = R1.ap[:, 0:H * hr].rearrange("p (h d) -> p h d", h=H)
        rb_ = R2.ap[:, 0:H * hr].rearrange("p (h d) -> p h d", h=H)
        K.op(DVE, lambda: dve.tensor_tensor(out=ra, in0=x1, in1=cb, op=ALU.mult), r=[T2] + rb, w=[R1])
        K.op(DVE, lambda: dve.tensor_tensor(out=rb_, in0=x2, in1=sb_, op=ALU.mult), r=[T2] + rb, w=[R2])
        K.op(DVE, lambda: dve.tensor_tensor(out=dv_[:, :, r0:r0 + hr], in0=ra, in1=rb_, op=ALU.subtract),
             r=[R1, R2, dst], w=[dst])
        K.op(DVE, lambda: dve.tensor_tensor(out=ra, in0=x1, in1=sb_, op=ALU.mult), r=[T2] + rb, w=[R1])
        K.op(DVE, lambda: dve.tensor_tensor(out=rb_, in0=x2, in1=cb, op=ALU.mult), r=[T2] + rb, w=[R2])
        K.op(DVE, lambda: dve.tensor_tensor(out=dv_[:, :, r0 + hr:r0 + 2 * hr], in0=ra, in1=rb_, op=ALU.add),
             r=[R1, R2, dst], w=[dst])

    pend = []
    opn = [0]

    def tr_piece(opc, dstT, h, qb):
        def f_():
            pt = psb[3]
            j_ = opn[0] % 8
            opn[0] += 1
            K.op(PE, lambda: pe.transpose(out=trv(pt)[:, j_, :], in_=opc.ap, identity=ident.ap), r=[opc, ident], w=[pt])
            evac_copy(dstT[:, h, qb * 128:(qb + 1) * 128], trv(pt)[:, j_, :], [pt], [dstT])
        pend.append(f_)

    def attention(es, heads, scale, finish, tagp):
        pts = [K.sb(es, tagp + "pT%d" % i, [128, 512], BF16) for i in range(4)]
        sbanks = [psb[0], psb[1], psb[2]]
        step = 0
        for hd in heads:
            hd["load"]()
            steps = []
            for qs in range(4):
                for kb in range(4 * qs + 4):
                    steps.append((qs, kb))

            def qk(i):
                qs, kb = steps[i]
                q0 = qs * 512 if kb < 4 * qs else kb * 128
                nq = (qs + 1) * 512 - q0
                ps = sbanks[i % 3]
                np_ = len(hd["parts"])
                for pi, (qt, kt, nr) in enumerate(hd["parts"]):
                    K.op(PE, lambda: pe.matmul(ps[:, 0:nq], lhsT=kt[0:nr, kb * 128:(kb + 1) * 128],
                                               rhs=qt[0:nr, q0:q0 + nq], start=(pi == 0), stop=(pi == np_ - 1)),
                         r=[qt, kt], w=[ps], inc=(pi == np_ - 1))
                return q0, nq, ps

            qq = [qk(0), qk(1)]
            for i, (qs, kb) in enumerate(steps):
                q0, nq, ps = qq.pop(0)
                if i + 2 < len(steps):
                    qq.append(qk(i + 2))
                for f_ in pend:
                    f_()
                pend.clear()
                pT = pts[step % 4]
                step += 1
                K.op(ACT, lambda: act.activation(out=pT[:, 0:nq], in_=ps[:, 0:nq], func=AF.Exp, scale=scale),
                     r=[ps], w=[pT])
                if kb >= 4 * qs:
                    K.op(POOL, lambda: pool.tensor_tensor(out=pT[:, 0:128], in0=pT[:, 0:128], in1=tri.ap, op=ALU.mult),
                         r=[pT, tri], w=[pT])
                for qb in range(q0 // 128, 4 * qs + 4):
                    acc = ps_acc[qb % 4]
                    c = qb * 128 - q0
                    K.op(PE, lambda: pe.matmul(acc[:, 0:130], lhsT=pT[:, c:c + 128], rhs=hd["v"][:, kb, :],
                                               start=(kb == 0), stop=(kb == qb)), r=[pT, hd["v"]], w=[acc])
                    if kb == qb:
                        finish(hd, qb, acc)
            for f_ in pend:
                f_()
            pend.clear()

    def transposeT(src, ncol_chunks, dstT, tt, nrows=128):
        for c8 in range((ncol_chunks + 7) // 8):
            n = min(8, ncol_chunks - c8 * 8)
            pt = ps_tr[c8 % 2]
            for j in range(n):
                ch = c8 * 8 + j
                K.op(PE, lambda: pe.transpose(out=trv(pt)[:, j, :], in_=src[:, ch * 128:(ch + 1) * 128], identity=ident.ap),
                     r=[src, ident], w=[pt], inc=(j == n - 1))
            evac_copy(dstT[:, c8 * 8:c8 * 8 + n, tt * 128:(tt + 1) * 128], trv(pt)[:, 0:n, :], [pt], [dstT])

    for b in [int(v) for v in os.environ.get("KB_LIST", "0,1").split(",")]:
        if stage < 1:
            break
        seq = ExitStack()
        with ExitStack() as es:
            hT = K.sb(es, "hT", [128, 16, S], BF16)
            with ExitStack() as es1:
                norm_to_T(es1, b, x_d, Buf("xin"), G1T, SH1, hT, "p1")
            K.barrier()
            if b == int(os.environ.get("KB_DUMP", "0")):
                K.dump("hT0", hT, BF16, [128, 16, S])
            ots = [K.sb(es, "ot%d" % i, [128, 512], F32) for i in range(3)]
            oc = [0]

            def evac_proj(ps, nb, c0, ncols, tt):
                ot = ots[oc[0] % 3]
                oc[0] += 1
                evac_copy(ot[:, 0:ncols], ps[:, 0:ncols], [ps], [ot])
                if nb == 0 and tt == 0 and b == 0:
                    K.dump("ot0", ot, F32, [128, 512])
                K.dma(STQ, proj_d[b][tt * 128:(tt + 1) * 128, c0:c0 + ncols], ot[:, 0:ncols], ot.ds, r=[ot],
                      wa=[B_proj[b]])
            linear(es, hT, 16, watt_d, ATT_IN, evac_proj, "p2")
        K.barrier()
        if stage < 3:
            seq.close()
            continue

        with ExitStack() as es:
            gq_lat = K.sb(es, "gq_lat", [128, 512], F32)
            gkv_lat = K.sb(es, "gkv_lat", [128, 512], F32)
            gq = K.sb(es, "gq", [128, 192], F32)
            gk = K.sb(es, "gk", [128, 192], F32)
            for t_, d_ in ((gq_lat, gq_lat_d), (gkv_lat, gkv_lat_d), (gq, gq_d), (gk, gk_d)):
                K.dma(SP, t_.ap, d_, t_.ds, w=[t_])
            cs, sn = rope_tables(es, b, inv_mla, 32, "m")
            wf = K.sb(es, "wf34", [128, 4, 2048], F32)
            wqb = K.sb(es, "wqb", [128, 4, 1536], BF16)
            wkvb = K.sb(es, "wkvb", [128, 4, 2048], BF16)
            K.dma(SP, wf[:, :, 0:1536], wqb_d.rearrange("(kc p) n -> p kc n", p=128), wf.ds, w=[wf])
            K.op(POOL, lambda: pool.tensor_copy(out=wqb.ap, in_=wf[:, :, 0:1536]), r=[wf], w=[wqb])
            K.dma(SP, wf.ap, wkvb_d.rearrange("(kc p) n -> p kc n", p=128), wf.ds, r=[], w=[wf])
            K.op(POOL, lambda: pool.tensor_copy(out=wkvb.ap, in_=wf.ap), r=[wf], w=[wkvb])
            lats = [K.sb(es, "lat%d" % i, [128, 1088], F32) for i in range(2)]
            latn = K.sb(es, "latn", [128, 1024], BF16)
            latT = K.sb(es, "latT", [128, 8, 128], BF16)
            junk = K.sb(es, "junk3", [128, 512], BF16)
            ss2 = K.sb(es, "ss2", [128, 2, 2], F32)
            q_sb = K.sb(es, "q_sb", [128, 1536], F32)
            kv_sb = K.sb(es, "kv_sb", [128, 2048], F32)
            k_sb = K.sb(es, "k_sb", [128, 1536], F32)
            T1 = K.sb(es, "T1", [128, 1536], F32)
            T2 = K.sb(es, "T2", [128, 1536], F32)
            R1 = K.sb(es, "R1", [128, 256], F32)
            R2 = K.sb(es, "R2", [128, 256], F32)
            SSq = K.sb(es, "SSq", [128, 8], F32)
            q_bf = K.sb(es, "q_bf", [128, 1536], BF16)
            k_bf = K.sb(es, "k_bf", [128, 1536], BF16)
            va_st = [K.sb(es, "va_st%d" % i, [128, 8, 130], BF16) for i in range(2)]
            qn_st = K.sb(es, "qn_st", [128, 8, 512], BF16)
            qr_st = K.sb(es, "qr_st", [64, 8, 512], BF16)
            kn_st = K.sb(es, "kn_st", [128, 8, 512], BF16)
            kr_st = K.sb(es, "kr_st", [64, 8, 512], BF16)
            for v_ in va_st:
                K.op(POOL, lambda: pool.memset(v_.ap, 1.0), w=[v_])
            for tt in range(NT):
                lat = lats[tt % 2]
                K.dma(SP, lat.ap, proj_d[b][tt * 128:(tt + 1) * 128, 0:1088], lat.ds, r=[B_proj[b]], w=[lat])
                for j, g_ in ((0, gq_lat), (1, gkv_lat)):
                    rms_stats([junk], lat[:, j * 512:(j + 1) * 512], 512, ss2[:, j, 0:1], junk.ap, [lat], ss2)
                    K.op(DVE, lambda: dve.scalar_tensor_tensor(out=latn[:, j * 512:(j + 1) * 512],
                                                               in0=lat[:, j * 512:(j + 1) * 512], scalar=ss2[:, j, 0:1],
                                                               in1=g_.ap, op0=ALU.mult, op1=ALU.mult),
                         r=[lat, ss2, g_], w=[latn])
                transposeT(latn, 8, latT, 0)
                cnt = 0
                for (dst, wb_, koff, nblk) in ((q_sb, wqb, 0, 3), (kv_sb, wkvb, 4, 4)):
                    for nb in range(nblk):
                        ps = ps_mm[cnt % 2]
                        cnt += 1
                        for kc in range(4):
                            K.op(PE, lambda: pe.matmul(ps.ap, lhsT=latT[:, koff + kc, :], rhs=wb_[:, kc, nb * 512:(nb + 1) * 512],
                                                       start=(kc == 0), stop=(kc == 3)), r=[latT, wb_], w=[ps], inc=(kc == 3))
                        evac_copy(dst[:, nb * 512:(nb + 1) * 512], ps.ap, [ps], [dst])
                kvv = kv_sb.ap.rearrange("p (h d) -> p h d", h=8)
                ksv = k_sb.ap.rearrange("p (h d) -> p h d", h=8)
                K.op(POOL, lambda: pool.tensor_copy(out=ksv[:, :, 0:128], in_=kvv[:, :, 0:128]), r=[kv_sb], w=[k_sb])
                K.op(POOL, lambda: pool.tensor_copy(out=ksv[:, :, 128:192],
                                                    in_=lat[:, 1024:1088].unsqueeze(1).to_broadcast([128, 8, 64])),
                     r=[lat, k_sb], w=[k_sb])
                va = va_st[tt % 2]
                K.op(POOL, lambda: pool.tensor_copy(out=va[:, :, 0:128], in_=kvv[:, :, 128:256]), r=[kv_sb], w=[va])
                K.dma(ACT, va_d[:, :, tt, :].rearrange("h p c -> p h c"), va.ap, va.ds, r=[va], wa=[B_qk])
                hn_rope(q_sb, 8, 192, gq, 128, 32, cs[:, tt, :], sn[:, tt, :], q_bf, T1, T2, R1, R2, SSq, [cs, sn])
                hn_rope(k_sb, 8, 192, gk, 128, 32, cs[:, tt, :], sn[:, tt, :], k_bf, T1, T2, R1, R2, SSq, [cs, sn])
                t4 = tt % 4
                for (src, nst, rst) in ((q_bf, qn_st, qr_st), (k_bf, kn_st, kr_st)):
                    sv3 = src.ap.rearrange("p (h d) -> p h d", h=8)
                    pt = ps_tr[0]
                    for h in range(8):
                        K.op(PE, lambda: pe.transpose(out=trv(pt)[:, h, :], in_=sv3[:, h, 0:128], identity=ident.ap),
                             r=[src, ident], w=[pt], inc=(h == 7))
                    evac_copy(nst[:, :, t4 * 128:(t4 + 1) * 128], trv(pt), [pt], [nst])
                    pt = ps_tr[1]
                    for h in range(8):
                        K.op(PE, lambda: pe.transpose(out=trv(pt)[0:64, h, :], in_=sv3[:, h, 128:192], identity=ident.ap),
                             r=[src, ident], w=[pt], inc=(h == 7))
                    evac_copy(rst[:, :, t4 * 128:(t4 + 1) * 128], trv(pt)[0:64, :, :], [pt], [rst])
                if t4 == 3:
                    t0 = (tt - 3) * 128
                    for (st_, dd, lo, hi) in ((qn_st, qT_d, 0, 128), (qr_st, qT_d, 128, 192), (kn_st, kT_d, 0, 128),
                                              (kr_st, kT_d, 128, 192)):
                        K.dma(ACT, dd[:, lo:hi, t0:t0 + 512].rearrange("h d t -> d h t"), st_.ap, st_.ds, r=[st_],
                              wa=[B_qk])
        K.barrier()
        if stage < 5:
            seq.close()
            continue

        mergedT = K.sb(seq, "mergedT", [128, 16, S], BF16)
        seq2 = ExitStack()
        seq.callback(seq2.close)
        oT = [K.sb(seq2, "oT%d" % i, [128, 8, S], BF16) for i in range(2)]
        with ExitStack() as es:
            opcs = [K.sb(es, "opc%d" % i, [128, 128], BF16) for i in range(4)]
            fcm = [0]
            hb = []
            for i in range(2):
                hb.append(dict(qn=K.sb(es, "aqn%d" % i, [128, S], BF16), qr=K.sb(es, "aqr%d" % i, [64, S], BF16),
                               kn=K.sb(es, "akn%d" % i, [128, S], BF16), kr=K.sb(es, "akr%d" % i, [64, S], BF16),
                               v=K.sb(es, "av%d" % i, [128, NT, 130], BF16)))
            recs = [K.sb(es, "rec%d" % i, [128, 1], F32) for i in range(4)]
            heads = []
            for h in range(8):
                bb = hb[h % 2]

                def load(h=h, bb=bb):
                    K.dma(SP, bb["qn"].ap, qT_d[h, 0:128, :], bb["qn"].ds, r=[B_qk], w=[bb["qn"]])
                    K.dma(SP, bb["qr"].ap, qT_d[h, 128:192, :], bb["qr"].ds, r=[B_qk], w=[bb["qr"]])
                    K.dma(SP, bb["kn"].ap, kT_d[h, 0:128, :], bb["kn"].ds, r=[B_qk], w=[bb["kn"]])
                    K.dma(SP, bb["kr"].ap, kT_d[h, 128:192, :], bb["kr"].ds, r=[B_qk], w=[bb["kr"]])
                    K.dma(SP, bb["v"].ap, va_d[h], bb["v"].ds, r=[B_qk], w=[bb["v"]])
                heads.append(dict(parts=[(bb["qn"], bb["kn"], 128), (bb["qr"], bb["kr"], 64)], v=bb["v"], h=h, load=load))

            def fin_mla(hd, qb, acc):
                rec = recs[qb % 4]
                K.op(DVE, lambda: dve.reciprocal(out=rec.ap, in_=acc[:, 128:129]), r=[acc], w=[rec])
                h = hd["h"]
                opc = opcs[fcm[0] % 4]
                fcm[0] += 1
                K.op(DVE, lambda: dve.tensor_scalar(out=opc.ap, in0=acc[:, 0:128], scalar1=rec[:, 0:1], scalar2=None, op0=ALU.mult),
                     r=[acc, rec], w=[opc])
                tr_piece(opc, oT[0], h, qb)
            attention(es, heads, 192 ** -0.5, fin_mla, "ma")
            if b == 0:
                K.dump("oaT", oT[0], BF16, [128, 8, S])
        K.barrier()
        if stage < 6:
            seq.close()
            continue

        with ExitStack() as es:
            gdq = K.sb(es, "gdq", [128, 64], F32)
            gdk = K.sb(es, "gdk", [128, 64], F32)
            K.dma(SP, gdq.ap, gdq_d, gdq.ds, w=[gdq])
            K.dma(SP, gdk.ap, gdk_d, gdk.ds, w=[gdk])
            cs, sn = rope_tables(es, b, inv_dif, 8, "d")
            dts = [K.sb(es, "dt%d" % i, [128, 3072], F32) for i in range(1)]
            T1 = K.sb(es, "dT1", [128, 1024], F32)
            T2 = K.sb(es, "dT2", [128, 1024], F32)
            R1 = K.sb(es, "dR1", [128, 128], F32)
            R2 = K.sb(es, "dR2", [128, 128], F32)
            SSq = K.sb(es, "dSSq", [128, 16], F32)
            dq_bf = K.sb(es, "dq_bf", [128, 1024], BF16)
            dk_bf = K.sb(es, "dk_bf", [128, 1024], BF16)
            vb_st = [K.sb(es, "vb_st%d" % i, [128, 8, 130], BF16) for i in range(2)]
            dq_st = K.sb(es, "dq_st", [64, 16, 256], BF16)
            dk_st = K.sb(es, "dk_st", [64, 16, 256], BF16)
            for v_ in vb_st:
                K.op(POOL, lambda: pool.memset(v_.ap, 1.0), w=[v_])
            for tt in range(NT):
                dt_ = dts[0]
                K.dma(SP, dt_.ap, proj_d[b][tt * 128:(tt + 1) * 128, 1088:4160], dt_.ds, r=[B_proj[b]], w=[dt_])
                vb = vb_st[tt % 2]
                K.op(POOL, lambda: pool.tensor_copy(out=vb[:, :, 0:128],
                                                    in_=dt_[:, 2048:3072].rearrange("p (h d) -> p h d", h=8)),
                     r=[dt_], w=[vb])
                K.dma(ACT, vb_d[:, :, tt, :].rearrange("h p c -> p h c"), vb.ap, vb.ds, r=[vb], wa=[B_dqk])
                t4 = tt % 2
                for (off, g_, dbf, dst_) in ((0, gdq, dq_bf, dq_st), (1024, gdk, dk_bf, dk_st)):
                    srcv = Tile_cols(dt_, off, 1024)
                    hn_rope(srcv, 16, 64, g_, 0, 8, cs[:, tt, :], sn[:, tt, :], dbf, T1, T2, R1, R2, SSq, [cs, sn])
                    sv3 = dbf.ap.rearrange("p (h d) -> p h d", h=16)
                    for c8 in range(2):
                        pt = ps_tr[c8]
                        for j in range(8):
                            K.op(PE, lambda: pe.transpose(out=trv(pt)[0:64, j, :], in_=sv3[:, c8 * 8 + j, :], identity=ident.ap),
                                 r=[dbf, ident], w=[pt], inc=(j == 7))
                        evac_copy(dst_[:, c8 * 8:(c8 + 1) * 8, t4 * 128:(t4 + 1) * 128], trv(pt)[0:64, :, :], [pt], [dst_])
                if t4 == 1:
                    t0 = (tt - 1) * 128
                    for (st_, dd) in ((dq_st, dqT_d), (dk_st, dkT_d)):
                        K.dma(ACT, dd[:, :, t0:t0 + 256].rearrange("h d t -> d h t"), st_.ap, st_.ds, r=[st_], wa=[B_dqk])
        K.barrier()

        with ExitStack() as es:
            opcs = [K.sb(es, "dopc%d" % i, [128, 128], BF16) for i in range(4)]
            gsub = K.sb(es, "gsub", [128, 128], F32)
            K.dma(SP, gsub.ap, gsub_d, gsub.ds, w=[gsub])
            K.op(DVE, lambda: dve.tensor_scalar(out=gsub.ap, in0=gsub.ap, scalar1=(1.0 - LAMBDA_INIT), scalar2=None,
                                                op0=ALU.mult), r=[gsub], w=[gsub])
            hb = []
            for i in range(2):
                hb.append(dict(q=K.sb(es, "dq%d" % i, [64, S], BF16), k=K.sb(es, "dk%d" % i, [64, S], BF16)))
            vts = [K.sb(es, "dv%d" % i, [128, NT, 130], BF16) for i in range(2)]
            recs = [K.sb(es, "drec%d" % i, [128, 1], F32) for i in range(4)]
            o1 = K.sb(es, "o1", [128, NT, 128], F32)
            ocs = [K.sb(es, "oc%d" % i, [128, 128], F32) for i in range(2)]
            oss = [K.sb(es, "oss%d" % i, [128, 1], F32) for i in range(2)]
            sqs = [K.sb(es, "sq7%d" % i, [128, 128], F32) for i in range(2)]
            heads = []
            for shh in range(16):
                bb = hb[shh % 2]
                vt = vts[(shh // 2) % 2]

                def load(shh=shh, bb=bb, vt=vt):
                    K.dma(SP, bb["q"].ap, dqT_d[shh], bb["q"].ds, r=[B_dqk], w=[bb["q"]])
                    K.dma(SP, bb["k"].ap, dkT_d[shh], bb["k"].ds, r=[B_dqk], w=[bb["k"]])
                    if shh % 2 == 0:
                        K.dma(SP, vt.ap, vb_d[shh // 2], vt.ds, r=[B_dqk], w=[vt])
                heads.append(dict(parts=[(bb["q"], bb["k"], 64)], v=vt, sh=shh, load=load))
            fc = [0]

            def fin_diff(hd, qb, acc):
                rec = recs[qb % 4]
                shh = hd["sh"]
                h = shh // 2
                K.op(DVE, lambda: dve.reciprocal(out=rec.ap, in_=acc[:, 128:129]), r=[acc], w=[rec])
                if shh % 2 == 0:
                    K.op(DVE, lambda: dve.tensor_scalar(out=o1[:, qb, :], in0=acc[:, 0:128], scalar1=rec[:, 0:1], scalar2=None,
                                                        op0=ALU.mult), r=[acc, rec], w=[o1])
                else:
                    oc_, os_ = ocs[fc[0] % 2], oss[fc[0] % 2]
                    sq_ = sqs[fc[0] % 2]
                    fc[0] += 1
                    K.op(DVE, lambda: dve.tensor_tensor(out=rec.ap, in0=rec.ap, in1=neglam.ap, op=ALU.mult),
                         r=[rec, neglam], w=[rec])
                    K.op(DVE, lambda: dve.scalar_tensor_tensor(out=oc_.ap, in0=acc[:, 0:128], scalar=rec[:, 0:1],
                                                               in1=o1[:, qb, :], op0=ALU.mult, op1=ALU.add),
                         r=[acc, rec, o1], w=[oc_])
                    K.op(POOL, lambda: pool.tensor_tensor(out=sq_.ap, in0=oc_.ap, in1=oc_.ap, op=ALU.mult), r=[oc_], w=[sq_])
                    K.op(DVE, lambda: dve.tensor_reduce(out=os_.ap, in_=sq_.ap, axis=AX.X, op=ALU.add), r=[sq_], w=[os_])
                    K.op(DVE, lambda: dve.tensor_scalar(out=os_.ap, in0=os_.ap, scalar1=1.0 / 128, scalar2=EPS, op0=ALU.mult,
                                                        op1=ALU.add), r=[os_], w=[os_])
                    K.op(POOL, lambda: pool.tensor_tensor(out=os_.ap, in0=os_.ap, in1=mhalf.ap, op=ALU.pow), r=[os_, mhalf], w=[os_])
                    opc = opcs[fc[0] % 4]
                    K.op(DVE, lambda: dve.scalar_tensor_tensor(out=opc.ap, in0=oc_.ap,
                                                               scalar=os_[:, 0:1], in1=gsub.ap, op0=ALU.mult, op1=ALU.mult),
                         r=[oc_, os_, gsub], w=[opc])
                    tr_piece(opc, oT[1], h, qb)
            attention(es, heads, 64 ** -0.5, fin_diff, "da")
            if b == 0:
                K.dump("obT", oT[1], BF16, [128, 8, S])
        K.barrier()
        if stage < 8:
            seq.close()
            continue

        with ExitStack() as es:
            wfs = [K.sb(es, "p8wf%d" % i, [128, 16, 512], F32) for i in range(1)]
            wbs = [K.sb(es, "p8wb%d" % i, [128, 16, 512], BF16) for i in range(1)]
            gts = [K.sb(es, "gt%d" % i, [128, 2, 512], F32) for i in range(2)]
            m1s = [K.sb(es, "m1%d" % i, [128, 512], F32) for i in range(2)]
            m2s = [K.sb(es, "m2%d" % i, [128, 512], F32) for i in range(2)]
            mbs = [K.sb(es, "mb%d" % i, [128, 512], BF16) for i in range(2)]
            n = 0
            pend8 = []
            def ld8(c0_):
                K.dma(SP, wfs[0][:, 0:8, :], wmo_d.rearrange("(kc p) n -> p kc n", p=128)[:, :, c0_:c0_ + 512], wfs[0].ds, w=[wfs[0]])
                K.dma(SP, wfs[0][:, 8:16, :], wdo_d.rearrange("(kc p) n -> p kc n", p=128)[:, :, c0_:c0_ + 512], wfs[0].ds, w=[wfs[0]])
            ld8(0)
            for nb in range(4):
                wf, wb = wfs[0], wbs[0]
                c0 = nb * 512
                K.op(POOL, lambda: pool.tensor_copy(out=wb.ap, in_=wf.ap), r=[wf], w=[wb])
                if nb + 1 < 4:
                    ld8(c0 + 512)
                for tt in range(NT):
                    gt, m1, m2, mb = gts[n % 2], m1s[n % 2], m2s[n % 2], mbs[n % 2]
                    n += 1
                    K.dma(SP, gt[:, 0, :], proj_d[b][tt * 128:(tt + 1) * 128, C_GA + c0:C_GA + c0 + 512], gt.ds,
                          r=[B_proj[b]], w=[gt])
                    K.dma(SP, gt[:, 1, :], proj_d[b][tt * 128:(tt + 1) * 128, C_GB + c0:C_GB + c0 + 512], gt.ds,
                          r=[B_proj[b]], w=[gt])
                    K.op(ACT, lambda: act.activation(out=gt.ap, in_=gt.ap, func=AF.Sigmoid), r=[gt], w=[gt])
                    pp = (ps_mm[0], ps_mm[1]) if (n % 2) else (ps_acc[0], ps_acc[1])
                    for br in range(2):
                        ps = pp[br]
                        for kc in range(8):
                            K.op(PE, lambda: pe.matmul(ps.ap, lhsT=oT[br][:, kc, tt * 128:(tt + 1) * 128],
                                                       rhs=wb[:, br * 8 + kc, :], start=(kc == 0), stop=(kc == 7)),
                                 r=[oT[br], wb], w=[ps], inc=(kc == 7))
                    while pend8:
                        pend8.pop(0)()
                    K.op(DVE, lambda: dve.tensor_tensor(out=m1.ap, in0=pp[0].ap, in1=gt[:, 0, :], op=ALU.mult),
                         r=[pp[0], gt], w=[m1])
                    K.op(DVE, lambda: dve.tensor_tensor(out=m2.ap, in0=pp[1].ap, in1=gt[:, 1, :], op=ALU.mult),
                         r=[pp[1], gt], w=[m2])
                    K.op(POOL, lambda: pool.tensor_tensor(out=mb.ap, in0=m1.ap, in1=m2.ap, op=ALU.add), r=[m1, m2], w=[mb])

                    def trm(mb=mb, nb=nb, tt=tt, pt=ps_tr[n % 2]):
                        for j in range(4):
                            K.op(PE, lambda: pe.transpose(out=trv(pt)[:, j, :], in_=mb[:, j * 128:(j + 1) * 128], identity=ident.ap),
                                 r=[mb, ident], w=[pt], inc=(j == 3))
                        K.op(ACT, lambda: act.copy(out=mergedT[:, nb * 4:nb * 4 + 4, tt * 128:(tt + 1) * 128], in_=trv(pt)[:, 0:4, :]),
                             r=[pt], w=[mergedT])
                    pend8.append(trm)
            while pend8:
                pend8.pop(0)()
        K.barrier()
        seq2.close()

        with ExitStack() as es:
            g1bc = K.sb(es, "g1bc", [128, D], F32)
            make_bc(g1bc, G1, b)
            xps = [K.sb(es, "xp%d" % i, [128, 512], F32) for i in range(3)]
            tps = [K.sb(es, "tp%d" % i, [128, 512], F32) for i in range(2)]
            n9 = [0]

            def evac_x1(ps, nb, c0, ncols, tt):
                xp, tp = xps[n9[0] % 3], tps[n9[0] % 2]
                n9[0] += 1
                K.dma(SP, xp.ap, x_d[b, tt * 128:(tt + 1) * 128, c0:c0 + 512], xp.ds, w=[xp])
                K.op(DVE, lambda: dve.tensor_tensor(out=tp.ap, in0=ps.ap, in1=g1bc[:, c0:c0 + 512], op=ALU.mult),
                     r=[ps, g1bc], w=[tp])
                K.op(POOL, lambda: pool.tensor_tensor(out=xp.ap, in0=xp.ap, in1=tp.ap, op=ALU.add), r=[xp, tp], w=[xp])
                K.dma(ACT, out_d[b, tt * 128:(tt + 1) * 128, c0:c0 + 512], xp.ap, xp.ds, r=[xp], wa=[B_out[b]])
            linear(es, mergedT, 16, wao_d, D, evac_x1, "p9")
        seq.close()
        K.barrier()
        if stage < 10:
            continue

        with ExitStack() as es:
            hT = K.sb(es, "h2T", [128, 16, S], BF16)
            with ExitStack() as es1:
                norm_to_T(es1, b, out_d, B_out[b], G2T, SH2, hT, "pa")
            K.barrier()
            K.dma(ACT, h2T_d, hT.ap, hT.ds, r=[hT], wa=[B_h2T])
            keysT = K.sb(es, "keysT", [128, 16, 128], F32)
            K.dma(SP, keysT.ap, keysT_d, keysT.ds, w=[keysT])
            wf = K.sb(es, "pawf", [128, 16, 512], F32)
            wbs = [K.sb(es, "pawb%d" % i, [128, 16, 512], BF16) for i in range(2)]
            pqs = [K.sb(es, "pq%d" % i, [128, 512], F32) for i in range(2)]
            sts = [K.sb(es, "subst%d" % i, [128, 4, 128], F32) for i in range(2)]
            n = 0
            load_w_block(wf, wbs[0], wpq_d, 16, 0, 512, POOL)
            for nb in range(4):
                wb = wbs[nb % 2]
                if nb + 1 < 4:
                    load_w_block(wf, wbs[(nb + 1) % 2], wpq_d, 16, (nb + 1) * 512, 512, POOL)
                for g in range(4):
                    gg = nb * 4 + g
                    for tb in range(4):
                        ps = ps_mm[n % 2]
                        pq = pqs[n % 2]
                        st_ = sts[n % 2]
                        n += 1
                        for kc in range(16):
                            K.op(PE, lambda: pe.matmul(ps.ap, lhsT=wb[:, kc, g * 128:(g + 1) * 128],
                                                       rhs=hT[:, kc, tb * 512:(tb + 1) * 512], start=(kc == 0), stop=(kc == 15)),
                                 r=[wb, hT], w=[ps], inc=(kc == 15))
                        K.op(ACT, lambda: act.copy(out=pq.ap, in_=ps.ap), r=[ps], w=[pq])
                        pt = ps_acc[n % 4]
                        for j in range(4):
                            K.op(PE, lambda: pe.matmul(pt[:, j * 128:(j + 1) * 128], lhsT=pq[:, j * 128:(j + 1) * 128],
                                                       rhs=keysT[:, gg, :], start=True, stop=True), r=[pq, keysT], w=[pt],
                                 inc=(j == 3))
                        K.op(DVE, lambda: dve.tensor_copy(out=st_.ap, in_=pt.ap.rearrange("p (j n) -> p j n", j=4)),
                             r=[pt], w=[st_])
                        K.dma(ACT, sub_d[tb * 512:(tb + 1) * 512, gg * 128:(gg + 1) * 128].rearrange("(j p) n -> p j n", p=128),
                              st_.ap, st_.ds, r=[st_], wa=[B_sub])
        K.barrier()

        with ExitStack() as es:
            g2bc = K.sb(es, "g2bc", [128, D], F32)
            make_bc(g2bc, G2, b)
            W_sb = K.sb(es, "W_sb", [128, 128, 256], BF16)
            h2b = K.sb(es, "h2b", [128, 16, 256], BF16)
            acc_sb = [K.sb(es, "acc_sb%d" % i, [128, D], F32) for i in range(2)]
            uts = [K.sb(es, "ut%d" % i, [128, 16, 128], BF16) for i in range(3)]
            GV = 4
            vgs = [K.sb(es, "vg%d" % i, [128, GV, D], BF16) for i in range(2)]
            a_sbs = [K.sb(es, "a_sb%d" % i, [128, 256], BF16) for i in range(2)]
            was = [K.sb(es, "wa%d" % i, [128, GV, 256], BF16) for i in range(2)]
            atmp = [K.sb(es, "atmp%d" % i, [128, 512], F32) for i in range(2)]
            subt = K.sb(es, "subt", [128, 16, 128], F32)
            subm = K.sb(es, "subm", [128, 16, 128], F32)
            sv = K.sb(es, "sv", [128, 16, 16], F32)
            si_u = K.sb(es, "si_u", [128, 16, 16], U32)
            si_f = K.sb(es, "si_f", [128, 16, 16], F32)
            cand = _TV(subm, subm.ap.rearrange("p g n -> p (g n)").rearrange("p (h c) -> p h c", h=8))
            candm = K.sb(es, "candm", [128, 8, 256], F32)
            topv = K.sb(es, "topv", [128, 8, 16], F32)
            pos_u = K.sb(es, "pos_u", [128, 8, 16], U32)
            ab_u = K.sb(es, "ab_u", [128, 2, 8, 16], U32)
            ab_f = K.sb(es, "ab_f", [128, 2, 8, 16], F32)
            eq = _TV(candm, candm.ap.rearrange("p h (a c) -> p h a c", a=16))
            ijg = K.sb(es, "ijg", [128, 3, 128], F32)
            gsum = K.sb(es, "gsum", [128, 8], F32)
            ijgTs = [[K.sb(es, "ijgT%d_%d" % (i, j), [128, 128], F32) for j in range(2)] for i in range(2)]
            GIs = [K.sb(es, "GI%d" % i, [128, 16, 128], BF16) for i in range(2)]
            OJs = [K.sb(es, "OJ%d" % i, [128, 16, 128], BF16) for i in range(2)]
            ijbs = [[K.sb(es, "ijb%d_%d" % (i, j), [128, 2, 128], BF16) for j in range(2)] for i in range(2)]
            iota_bf = K.sb(es, "iota_bf", [128, 128], BF16)
            K.op(DVE, lambda: dve.tensor_copy(out=iota_bf.ap, in_=iota.ap), r=[iota], w=[iota_bf])
            xo = _TV(subt, subt.ap.rearrange("p g n -> p (g n)"))
            xo.ds = subt.ds
            ucnt = [0]
            tcnt = [0]
            NBLK = S // 256

            def make_sel(blk):
                th = []

                def T(E, meth, r, w, **kw):
                    th.append(lambda: K.op(E, lambda: getattr(E.e, meth)(**kw), r=r, w=w))
                for st in range(2):
                    r0_ = blk * 256 + st * 128
                    src_ = sub_d[r0_:r0_ + 128, :]
                    th.append(lambda src_=src_: K.dma(SP, subt.ap.rearrange("p g n -> p (g n)"), src_, subt.ds, r=[B_sub], w=[subt]))
                    for g in range(16):
                        T(DVE, "max", [subt], [sv], out=sv[:, g, 0:8], in_=subt[:, g, :])
                        T(DVE, "max_index", [subt, sv], [si_u], out=si_u[:, g, 0:8], in_max=sv[:, g, 0:8], in_values=subt[:, g, :])
                        T(DVE, "match_replace", [subt, sv], [subm], out=subm[:, g, :], in_to_replace=sv[:, g, 0:8],
                          in_values=subt[:, g, :], imm_value=NEG)
                        T(DVE, "max", [subm], [sv], out=sv[:, g, 8:16], in_=subm[:, g, :])
                        T(DVE, "max_index", [subm, sv], [si_u], out=si_u[:, g, 8:16], in_max=sv[:, g, 8:16], in_values=subm[:, g, :])
                    T(DVE, "tensor_copy", [si_u], [si_f], out=si_f.ap, in_=si_u.ap)
                    sv4 = sv.ap.rearrange("p (h two) k -> p h two k", two=2)
                    si4 = si_f.ap.rearrange("p (h two) k -> p h two k", two=2)
                    T(DVE, "tensor_tensor", [sv], [cand], out=cand.ap.rearrange("p h (a c) -> p h a c", a=16),
                      in0=sv4[:, :, 0, :].unsqueeze(3).to_broadcast([128, 8, 16, 16]),
                      in1=sv4[:, :, 1, :].unsqueeze(2).to_broadcast([128, 8, 16, 16]), op=ALU.add)
                    for h in range(8):
                        T(DVE, "max", [cand], [topv], out=topv[:, h, 0:8], in_=cand[:, h, :])
                        T(DVE, "max_index", [cand, topv], [pos_u], out=pos_u[:, h, 0:8], in_max=topv[:, h, 0:8], in_values=cand[:, h, :])
                        T(DVE, "match_replace", [cand, topv], [candm], out=candm[:, h, :], in_to_replace=topv[:, h, 0:8],
                          in_values=cand[:, h, :], imm_value=NEG)
                        T(DVE, "max", [candm], [topv], out=topv[:, h, 8:16], in_=candm[:, h, :])
                        T(DVE, "max_index", [candm, topv], [pos_u], out=pos_u[:, h, 8:16], in_max=topv[:, h, 8:16], in_values=candm[:, h, :])
                    T(DVE, "tensor_single_scalar", [pos_u], [ab_u], out=ab_u[:, 0], in_=pos_u.ap, scalar=4, op=ALU.logical_shift_right)
                    T(DVE, "tensor_single_scalar", [pos_u, ab_u], [ab_u], out=ab_u[:, 1], in_=pos_u.ap, scalar=15, op=ALU.bitwise_and)
                    T(DVE, "tensor_copy", [ab_u], [ab_f], out=ab_f.ap, in_=ab_u.ap)
                    ijv = ijg.ap.rearrange("p c (h k) -> p c h k", h=8)
                    for w_ in range(2):
                        T(DVE, "tensor_tensor", [ab_f, iota], [eq], out=eq.ap, in0=ab_f[:, w_].unsqueeze(3).to_broadcast([128, 8, 16, 16]),
                          in1=iota[:, 0:16].unsqueeze(1).unsqueeze(1).to_broadcast([128, 8, 16, 16]), op=ALU.is_equal)
                        T(DVE, "tensor_tensor", [eq, si_f], [eq], out=eq.ap, in0=eq.ap,
                          in1=si4[:, :, w_, :].unsqueeze(2).to_broadcast([128, 8, 16, 16]), op=ALU.mult)
                        T(DVE, "tensor_reduce", [eq], [ijg], out=ijv[:, w_], in_=eq.ap, axis=AX.X, op=ALU.add)
                    T(DVE, "tensor_tensor", [topv, ijg], [ijg], out=ijv[:, 2], in0=topv.ap,
                      in1=topv[:, :, 0:1].to_broadcast([128, 8, 16]), op=ALU.subtract)
                    T(ACT, "activation", [ijg], [ijg], out=ijg[:, 2, :], in_=ijg[:, 2, :], func=AF.Exp)
                    T(DVE, "tensor_reduce", [ijg], [gsum], out=gsum.ap, in_=ijv[:, 2], axis=AX.X, op=ALU.add)
                    T(DVE, "reciprocal", [gsum], [gsum], out=gsum.ap, in_=gsum.ap)
                    T(DVE, "tensor_tensor", [ijg, gsum], [ijg], out=ijv[:, 2], in0=ijv[:, 2],
                      in1=gsum.ap.unsqueeze(2).to_broadcast([128, 8, 16]), op=ALU.mult)
                    dstT = ijgTs[blk % 2][st]
                    dstB = ijbs[blk % 2][st]

                    def trs(dstT=dstT, dstB=dstB):
                        ptf = ps_tr[0]
                        for c in range(3):
                            K.op(PE, lambda: pe.transpose(out=ptf[:, c * 128:(c + 1) * 128], in_=ijg[:, c, :], identity=identf.ap),
                                 r=[ijg, identf], w=[ptf], inc=(c == 2))
                        K.op(ACT, lambda: act.copy(out=dstT.ap, in_=ptf[:, 256:384]), r=[ptf], w=[dstT])
                        K.op(ACT, lambda: act.copy(out=dstB.ap, in_=ptf[:, 0:256].rearrange("p (c t) -> p c t", c=2)),
                             r=[ptf], w=[dstB])
                    th.append(trs)
                return th

            def w_build(blk):
                cn = 0
                for st in range(2):
                    ijgT = ijgTs[blk % 2][st]
                    ijb = ijbs[blk % 2][st]
                    for hf in range(8):
                        tsl = slice(hf * 16, (hf + 1) * 16)
                        OJ_, GI_ = OJs[cn % 2], GIs[cn % 2]
                        cn += 1
                        K.op(DVE, lambda: dve.tensor_tensor(out=OJ_.ap, in0=iota_bf.ap.unsqueeze(1).to_broadcast([128, 16, 128]),
                                                            in1=ijb[:, 1, tsl].unsqueeze(2).to_broadcast([128, 16, 128]),
                                                            op=ALU.is_equal), r=[iota_bf, ijb], w=[OJ_])
                        K.op(DVE, lambda: dve.tensor_tensor(out=GI_.ap, in0=iota_bf.ap.unsqueeze(1).to_broadcast([128, 16, 128]),
                                                            in1=ijb[:, 0, tsl].unsqueeze(2).to_broadcast([128, 16, 128]),
                                                            op=ALU.is_equal), r=[iota_bf, ijb], w=[GI_])
                        K.op(POOL, lambda: pool.tensor_tensor(out=GI_.ap, in0=GI_.ap,
                                                              in1=ijgT[:, tsl].unsqueeze(2).to_broadcast([128, 16, 128]),
                                                              op=ALU.mult), r=[GI_, ijgT], w=[GI_])
                        for t4 in range(4):
                            pw = ps_tr[1] if (t4 % 2) else ps_tr[0]
                            for j in range(4):
                                tl = t4 * 4 + j
                                K.op(PE, lambda: pe.matmul(pw[:, j * 128:(j + 1) * 128], lhsT=OJ_[:, tl, :], rhs=GI_[:, tl, :],
                                                           start=True, stop=True), r=[OJ_, GI_], w=[pw], inc=(j == 3))
                            tg = st * 128 + hf * 16 + t4 * 4
                            K.op(ACT, lambda: act.copy(out=W_sb[:, :, tg:tg + 4], in_=pw.ap.rearrange("p (t i) -> p i t", t=4)),
                                 r=[pw], w=[W_sb])

            def a_half(grp, half):
                vg = vgs[grp % 2]
                wa = was[grp % 2]
                for ii in (2 * half, 2 * half + 1):
                    i = grp * GV + ii
                    ut = uts[ucnt[0] % 3]
                    a_sb = a_sbs[ucnt[0] % 2]
                    pa = psb[0] if (ucnt[0] % 2 == 0) else psb[1]
                    pav = pa[:, 0:256]
                    ucnt[0] += 1
                    K.dma(SP, ut.ap.rearrange("p c j -> p (c j)"), ub_d[i], ut.ds, r=[B_tab], w=[ut])
                    for c in range(16):
                        K.op(PE, lambda: pe.matmul(pav, lhsT=ut[:, c, :], rhs=h2b[:, c, :], start=(c == 0), stop=(c == 15)),
                             r=[ut, h2b], w=[pa], inc=(c == 15))
                    K.op(ACT, lambda: act.activation(out=a_sb.ap, in_=pav, func=AF.Gelu), r=[pa], w=[a_sb])
                    K.op(DVE, lambda: dve.tensor_tensor(out=wa[:, ii, :], in0=a_sb.ap, in1=W_sb[:, i, :], op=ALU.mult),
                         r=[a_sb, W_sb], w=[wa])

            def v_load(grp):
                vg = vgs[grp % 2]
                K.dma(SP, vg.ap, vbf_d[grp * GV:(grp + 1) * GV].rearrange("i j d -> j i d"), vg.ds, r=[B_tab], w=[vg])

            def v_half(grp, st):
                vg = vgs[grp % 2]
                wa = was[grp % 2]
                for ii in range(GV):
                    for n_ in range(4):
                        K.op(PE, lambda: pe.matmul(ps_acc[n_].ap, lhsT=wa[:, ii, st * 128:(st + 1) * 128],
                                                   rhs=vg[:, ii, n_ * 512:(n_ + 1) * 512], start=(ii == 0),
                                                   stop=(ii == GV - 1)), r=[wa, vg], w=[ps_acc[n_]], inc=(ii == GV - 1))
                for n_ in range(4):
                    dst = acc_sb[st][:, n_ * 512:(n_ + 1) * 512]
                    if grp == 0:
                        K.op(ACT, lambda: act.copy(out=dst, in_=ps_acc[n_].ap), r=[ps_acc[n_]], w=[acc_sb[st]])
                    else:
                        tm = atmp[tcnt[0] % 2]
                        tcnt[0] += 1
                        K.op(ACT, lambda: act.copy(out=tm.ap, in_=ps_acc[n_].ap), r=[ps_acc[n_]], w=[tm])
                        K.op(POOL, lambda: pool.tensor_tensor(out=dst, in0=dst, in1=tm.ap, op=ALU.add),
                             r=[tm, acc_sb[st]], w=[acc_sb[st]])

            ngrp = 128 // GV
            for f_ in make_sel(0):
                f_()
            w_build(0)
            for blk in range(NBLK):
                t0 = blk * 256
                if dbg and b == 0 and blk == 0:
                    pass
                K.dma(SP, h2b.ap, h2T_d[:, :, t0:t0 + 256], h2b.ds, r=[B_h2T], w=[h2b])
                th = make_sel(blk + 1) if blk + 1 < NBLK else []
                per = (len(th) + ngrp - 1) // ngrp + 1
                v_load(0)
                a_half(0, 0)
                a_half(0, 1)
                for grp in range(ngrp):
                    if grp + 1 < ngrp:
                        v_load(grp + 1)
                    v_half(grp, 0)
                    if grp + 1 < ngrp:
                        a_half(grp + 1, 0)
                    v_half(grp, 1)
                    if grp + 1 < ngrp:
                        a_half(grp + 1, 1)
                    for _ in range(min(per, len(th))):
                        th.pop(0)()
                while th:
                    th.pop(0)()
                if blk + 1 < NBLK:
                    w_build(blk + 1)
                for st in range(2):
                    r0_ = t0 + st * 128
                    K.dma(SP, xo.ap, out_d[b, r0_:r0_ + 128, :], xo.ds, r=[B_out[b]], w=[xo])
                    K.op(DVE, lambda: dve.tensor_tensor(out=acc_sb[st].ap, in0=acc_sb[st].ap, in1=g2bc.ap, op=ALU.mult),
                         r=[acc_sb[st], g2bc], w=[acc_sb[st]])
                    K.op(POOL, lambda: pool.tensor_tensor(out=xo.ap, in0=xo.ap, in1=acc_sb[st].ap, op=ALU.add),
                         r=[xo, acc_sb[st]], w=[xo])
                    K.dma(ACT, out_d[b, r0_:r0_ + 128, :], xo.ap, xo.ds, r=[xo], wa=[Buf("fin")])
        K.barrier()
    K.barrier()
    top.close()
    return nc


class _TV:
    def __init__(self, parent, ap):
        self.ap = ap
        self.buf = parent.buf

    def __getitem__(self, k):
        return self.ap[k]


def Tile_view(t, tt):
    return _TV(t, t.ap[:, tt, :])


def Tile_cols(t, off, n):
    return _TV(t, t.ap[:, off:off + n])


DBG_OUT = set()
DBG = {}


def prep_shared(inp):
    f = np.float32
    sh = {}
    sh["n1wT"] = np.ascontiguousarray(inp["norm1_w"][0].reshape(16, 128).T)
    sh["n2wT"] = np.ascontiguousarray(inp["norm2_w"][0].reshape(16, 128).T)
    sh["w_ada"] = np.ascontiguousarray(inp["w_ada"][0])
    sh["b_adaT"] = np.ascontiguousarray(inp["b_ada"][0].reshape(96, 128).T)
    sh["w_att_in"] = np.ascontiguousarray(inp["w_att_in"][0])
    rep = lambda v: np.ascontiguousarray(np.broadcast_to(v[None], (128,) + v.shape))
    sh["mla_q_norm_bc"] = rep(inp["mla_q_norm"][0])
    sh["mla_kv_norm_bc"] = rep(inp["mla_kv_norm"][0])
    sh["w_mla_qb"] = np.ascontiguousarray(inp["w_mla_qb"][0])
    sh["w_mla_kvb"] = np.ascontiguousarray(inp["w_mla_kvb"][0])
    sh["mla_qk_norm_q_bc"] = rep(inp["mla_qk_norm_q"][0])
    sh["mla_qk_norm_k_bc"] = rep(inp["mla_qk_norm_k"][0])
    sh["w_mla_o"] = np.ascontiguousarray(inp["w_mla_o"][0])
    sh["diff_q_norm_bc"] = rep(inp["diff_q_norm"][0])
    sh["diff_k_norm_bc"] = rep(inp["diff_k_norm"][0])
    sh["diff_lambda_bc"] = rep(inp["diff_lambda"][0])
    sh["diff_subln_bc"] = rep(inp["diff_subln"][0])
    sh["w_diff_o"] = np.ascontiguousarray(inp["w_diff_o"][0])
    sh["w_att_out"] = np.ascontiguousarray(inp["w_att_out"][0])
    sh["w_peer_q"] = np.ascontiguousarray(inp["w_peer_q"][0])
    sh["peer_keysT"] = np.ascontiguousarray(inp["peer_keys"][0].reshape(16, 128, 128).transpose(2, 0, 1))
    u = inp["peer_u"][0].reshape(128, 128, 16, 128)
    sh["peer_uT"] = np.ascontiguousarray(u.transpose(0, 3, 2, 1)).reshape(128, 128, 2048)
    sh["peer_v"] = np.ascontiguousarray(inp["peer_v"][0].reshape(128, 128, 2048))
    sh["iota128"] = rep(np.arange(128, dtype=f))
    theta = 500000.0
    sh["inv_mla"] = rep((theta ** (-np.arange(0, 64, 2, dtype=f) / f(64))).astype(f))
    sh["inv_dif"] = rep((theta ** (-np.arange(0, 16, 2, dtype=f) / f(16))).astype(f))
    return sh


def prep_core(inp, c):
    b0 = c * NB
    m = {}
    m["x"] = np.ascontiguousarray(inp["x"][b0:b0 + NB])
    m["cT"] = np.ascontiguousarray(inp["c"][b0:b0 + NB].reshape(NB, 16, 128).transpose(2, 1, 0))
    pos = inp["positions"][b0:b0 + NB]
    m["posT"] = np.ascontiguousarray(pos.reshape(NB, NT, 128).transpose(2, 0, 1))
    return m


def kernel(**inputs):
    inp = {k: np.asarray(v) for k, v in inputs.items()}
    nc = build_nc()
    sh = prep_shared(inp)
    in_maps = []
    for c in range(8):
        m = dict(sh)
        m.update(prep_core(inp, c))
        in_maps.append(m)
    res = run_bass_kernel_spmd(nc, in_maps, core_ids=list(range(8)))
    return np.concatenate([r["out"] for r in res.results], axis=0).astype(np.float32)
```

```python
import os, math
from contextlib import ExitStack
import numpy as np
import concourse.bass as bass
import concourse.mybir as mybir
from concourse.bass_utils import run_bass_kernel_spmd

F32 = mybir.dt.float32
BF16 = mybir.dt.bfloat16
U32 = mybir.dt.uint32
I32 = mybir.dt.int32
AF = mybir.ActivationFunctionType
ALU = mybir.AluOpType
AX = mybir.AxisListType

NB = 2
S = 2048
D = 2048
NT = S // 128
ATT_IN = 8256
EPS = 1e-6
LAMBDA_INIT = 0.8 - 0.6 * math.exp(-0.3 * 0)
C_GA = 4160
C_GB = 6208
NEG = -1.0e30


class Sem:
    def __init__(self, nc, name, dma=False):
        self.h = nc.semaphore(name).__enter__()
        self.name = name
        self.cnt = 0
        self.dma = dma


class Tk:
    __slots__ = ("sem", "val")

    def __init__(self, sem, val):
        self.sem = sem
        self.val = val


class Buf:
    def __init__(self, name):
        self.name = name
        self.w = {}
        self.r = {}


class Eng:
    def __init__(self, nc, e, name):
        self.e = e
        self.name = name
        self.sem = Sem(nc, "s_" + name)
        self.waited = {}
        self.is_pe = name == "pe"

    def wait_for(self, tk):
        val = tk.sem.cnt if tk.sem.dma else tk.val
        if self.waited.get(tk.sem.name, 0) >= val:
            return
        self.e.wait_ge(tk.sem.h, val)
        self.waited[tk.sem.name] = val


class Tile:
    def __init__(self, K, h, name):
        self.h = h
        self.ap = h.ap()
        self.buf = Buf(name)
        self.K = K
        self._ds = None

    def __getitem__(self, k):
        return self.ap[k]

    @property
    def ds(self):
        if self._ds is None:
            self._ds = self.K.next_ds()
        return self._ds


class KB:
    def __init__(self, nc):
        self.nc = nc
        self.PE = Eng(nc, nc.tensor, "pe")
        self.ACT = Eng(nc, nc.scalar, "act")
        self.DVE = Eng(nc, nc.vector, "dve")
        self.POOL = Eng(nc, nc.gpsimd, "pool")
        self.SP = Eng(nc, nc.sync, "sp")
        self.engs = [self.PE, self.ACT, self.DVE, self.POOL, self.SP]
        self.dsems = [Sem(nc, "d%d" % i, dma=True) for i in range(56)]
        self.ds_i = 0

    def next_ds(self):
        s = self.dsems[self.ds_i % len(self.dsems)]
        self.ds_i += 1
        return s

    def sb(self, es, name, shape, dt):
        self.nm = getattr(self, "nm", 0) + 1
        name = "t%d_%s" % (self.nm, name)
        h = es.enter_context(self.nc.sbuf_tensor(name, list(shape), dt))
        return Tile(self, h, name)

    def _deps(self, E, r, w):
        for b in r:
            b = getattr(b, "buf", b)
            for tk in b.w.values():
                if not (E.is_pe and tk.sem is E.sem):
                    E.wait_for(tk)
        for b in w:
            b = getattr(b, "buf", b)
            for tk in list(b.w.values()) + list(b.r.values()):
                if not (E.is_pe and tk.sem is E.sem):
                    E.wait_for(tk)

    def _upd(self, tk, r, w, wa=()):
        for b in r:
            b = getattr(b, "buf", b)
            b.r[tk.sem.name] = tk
        for b in w:
            b = getattr(b, "buf", b)
            b.w = {tk.sem.name: tk}
            b.r = {}
        for b in wa:
            b = getattr(b, "buf", b)
            b.w[tk.sem.name] = tk

    def op(self, E, fn, r=(), w=(), inc=True):
        self._deps(E, r, w)
        inst = fn()
        if inc:
            E.sem.cnt += 1
            inst.then_inc(E.sem.h, 1)
            tk = Tk(E.sem, E.sem.cnt)
        else:
            tk = Tk(E.sem, E.sem.cnt + 1)
        self._upd(tk, r, w)
        return tk

    def dma(self, Q, out, in_, ds, r=(), w=(), wa=(), **kw):
        self._deps(Q, r, list(w) + list(wa))
        Q.e.dma_start(out=out, in_=in_, **kw).then_inc(ds.h, 16)
        ds.cnt += 16
        tk = Tk(ds, ds.cnt)
        self._upd(tk, r, w, wa)
        return tk

    def dump(self, name, t, dt, shape):
        if not self.dbg:
            return
        d = self.nc.dram_tensor("dbg_" + name, list(shape), dt, kind="ExternalOutput").ap()
        self.dma(self.SP, d, t.ap, t.ds, r=[t])

    def barrier(self):
        sems = [e.sem for e in self.engs] + self.dsems
        for E in self.engs:
            for s in sems:
                if s.cnt > 0 and E.waited.get(s.name, 0) < s.cnt:
                    E.e.wait_ge(s.h, s.cnt)
                    E.waited[s.name] = s.cnt


def build_nc(stage=99, dbg=False):
    nc = bass.Bass("TRN2", target_bir_lowering=False)
    K = KB(nc)
    K.dbg = dbg
    PE, ACT, DVE, POOL, SP = K.PE, K.ACT, K.DVE, K.POOL, K.SP
    pe, act, dve, pool = nc.tensor, nc.scalar, nc.vector, nc.gpsimd
    STQ = SP if os.environ.get("KB_STQ", "sp") == "sp" else POOL

    def din(name, shape, dt=F32):
        return nc.dram_tensor(name, list(shape), dt, kind="ExternalInput").ap()

    def dscr(name, shape, dt=F32):
        kind = "ExternalOutput" if (dbg and name in DBG_OUT) else "Internal"
        return nc.dram_tensor(name, list(shape), dt, kind=kind).ap()

    x_d = din("x", [NB, S, D])
    cT_d = din("cT", [128, 16, NB])
    pos_d = din("posT", [128, NB, NT], I32)
    n1w_d = din("n1wT", [128, 16])
    n2w_d = din("n2wT", [128, 16])
    wada_d = din("w_ada", [D, 6 * D])
    bada_d = din("b_adaT", [128, 96])
    watt_d = din("w_att_in", [D, ATT_IN])
    gq_lat_d = din("mla_q_norm_bc", [128, 512])
    gkv_lat_d = din("mla_kv_norm_bc", [128, 512])
    wqb_d = din("w_mla_qb", [512, 1536])
    wkvb_d = din("w_mla_kvb", [512, 2048])
    gq_d = din("mla_qk_norm_q_bc", [128, 192])
    gk_d = din("mla_qk_norm_k_bc", [128, 192])
    wmo_d = din("w_mla_o", [1024, D])
    gdq_d = din("diff_q_norm_bc", [128, 64])
    gdk_d = din("diff_k_norm_bc", [128, 64])
    dlam_d = din("diff_lambda_bc", [128, 4, 64])
    gsub_d = din("diff_subln_bc", [128, 128])
    wdo_d = din("w_diff_o", [1024, D])
    wao_d = din("w_att_out", [D, D])
    wpq_d = din("w_peer_q", [D, D])
    keysT_d = din("peer_keysT", [128, 16, 128])
    uT_d = din("peer_uT", [128, 128, 2048])
    v_d = din("peer_v", [128, 128, 2048])
    iota_d = din("iota128", [128, 128])
    inv_mla_d = din("inv_mla", [128, 32])
    inv_dif_d = din("inv_dif", [128, 8])
    out_d = nc.dram_tensor("out", [NB, S, D], F32, kind="ExternalOutput").ap()

    proj_d = [dscr("proj%d" % i, [S, ATT_IN]) for i in range(NB)]
    if os.environ.get("KB_SWAP"):
        proj_d = proj_d[::-1]
    qT_d = dscr("qT", [8, 192, S], BF16)
    kT_d = dscr("kT", [8, 192, S], BF16)
    va_d = dscr("va", [8, 128, NT, 130], BF16)
    dqT_d = dscr("dqT", [16, 64, S], BF16)
    dkT_d = dscr("dkT", [16, 64, S], BF16)
    vb_d = dscr("vb", [8, 128, NT, 130], BF16)
    h2T_d = dscr("h2T", [128, 16, S], BF16)
    sub_d = dscr("sub", [S, 2048])
    ub_d = dscr("ub", [128, 128, 2048], BF16)
    vbf_d = dscr("vbf", [128, 128, 2048], BF16)
    B_proj = [Buf("proj0"), Buf("proj1")]
    B_qk = Buf("qkscr")
    B_dqk = Buf("dqkscr")
    B_h2T = Buf("h2Tscr")
    B_sub = Buf("subscr")
    B_tab = Buf("tabscr")
    B_out = [Buf("out0"), Buf("out1")]

    psb = []
    for i in range(8):
        h = nc.alloc_psum_tensor("psb%d" % i, [128, 512], F32)
        t = Tile(K, h, "psb%d" % i)
        psb.append(t)
    ps_mm = psb[0:2]
    ps_tr = psb[2:4]
    ps_acc = psb[4:8]

    def trv(t):
        return t.ap.bitcast(BF16).rearrange("p (a b) -> p a b", a=8)

    top = ExitStack()
    ident = K.sb(top, "ident", [128, 128], BF16)
    identf = K.sb(top, "identf", [128, 128], F32)
    tri = K.sb(top, "tri", [128, 128], BF16)
    modT = K.sb(top, "modT", [128, 96, NB], F32)
    G1T = K.sb(top, "G1T", [128, 16, NB], F32)
    G2T = K.sb(top, "G2T", [128, 16, NB], F32)
    neglam = K.sb(top, "neglam", [128, 1], F32)
    iota = K.sb(top, "iota", [128, 128], F32)
    posf = K.sb(top, "posf", [128, NB, NT], F32)
    inv_mla = K.sb(top, "inv_mla", [128, 32], F32)
    inv_dif = K.sb(top, "inv_dif", [128, 8], F32)

    modP = [K.sb(top, "modP%d" % i, [128, 96, 2], F32) for i in range(NB)]
    GP1 = [K.sb(top, "GP1%d" % i, [128, 16, 2], F32) for i in range(NB)]
    GP2 = [K.sb(top, "GP2%d" % i, [128, 16, 2], F32) for i in range(NB)]
    epsc = K.sb(top, "epsc", [128, 1], F32)
    mhalf = K.sb(top, "mhalf", [128, 1], F32)
    K.op(POOL, lambda: pool.memset(mhalf.ap, -0.5), w=[mhalf])
    K.op(POOL, lambda: pool.memset(epsc.ap, EPS), w=[epsc])
    for t_ in (ident, identf):
        K.op(POOL, lambda: pool.memset(t_.ap, 1.0), w=[t_])
        K.op(POOL, lambda: pool.affine_select(out=t_.ap, in_=t_.ap, pattern=[[-1, 128]], compare_op=ALU.is_equal,
                                              fill=0.0, base=0, channel_multiplier=1), r=[t_], w=[t_])
    K.op(POOL, lambda: pool.memset(tri.ap, 1.0), w=[tri])
    K.op(POOL, lambda: pool.affine_select(out=tri.ap, in_=tri.ap, pattern=[[1, 128]], compare_op=ALU.is_ge,
                                          fill=0.0, base=0, channel_multiplier=-1), r=[tri], w=[tri])
    K.dma(SP, iota.ap, iota_d, iota.ds, w=[iota])
    K.dma(SP, inv_mla.ap, inv_mla_d, inv_mla.ds, w=[inv_mla])
    K.dma(SP, inv_dif.ap, inv_dif_d, inv_dif.ds, w=[inv_dif])
    posi = K.sb(top, "posi", [128, NB, NT], I32)
    K.dma(SP, posi.ap, pos_d, posi.ds, w=[posi])
    K.op(DVE, lambda: dve.tensor_copy(out=posf.ap, in_=posi.ap), r=[posi], w=[posf])

    with ExitStack() as es:
        cT = K.sb(es, "cT", [128, 16, NB], F32)
        scT = K.sb(es, "scT", [128, 16, NB], F32)
        bada = K.sb(es, "bada", [128, 96], F32)
        n1w = K.sb(es, "n1w", [128, 16], F32)
        n2w = K.sb(es, "n2w", [128, 16], F32)
        dl = K.sb(es, "dl", [128, 4, 64], F32)
        dl2 = K.sb(es, "dl2", [128, 2, 64], F32)
        ls = K.sb(es, "ls", [128, 2], F32)
        wts = [K.sb(es, "wada%d" % i, [128, 16, 512], F32) for i in range(2)]
        K.dma(SP, cT.ap, cT_d, cT.ds, w=[cT])
        K.dma(SP, bada.ap, bada_d, bada.ds, w=[bada])
        K.dma(SP, n1w.ap, n1w_d, n1w.ds, w=[n1w])
        K.dma(SP, n2w.ap, n2w_d, n2w.ds, w=[n2w])
        K.dma(SP, dl.ap, dlam_d, dl.ds, w=[dl])
        K.op(ACT, lambda: act.activation(out=scT.ap, in_=cT.ap, func=AF.Silu), r=[cT], w=[scT])
        K.op(DVE, lambda: dve.tensor_tensor(out=dl2.ap, in0=dl.ap.rearrange("p (a b) d -> p a b d", b=2)[:, :, 0, :],
                                            in1=dl.ap.rearrange("p (a b) d -> p a b d", b=2)[:, :, 1, :], op=ALU.mult),
             r=[dl], w=[dl2])
        K.op(DVE, lambda: dve.tensor_reduce(out=ls.ap, in_=dl2.ap, axis=AX.X, op=ALU.add), r=[dl2], w=[ls])
        K.op(ACT, lambda: act.activation(out=ls.ap, in_=ls.ap, func=AF.Exp), r=[ls], w=[ls])
        K.op(DVE, lambda: dve.tensor_tensor(out=neglam.ap, in0=ls[:, 1:2], in1=ls[:, 0:1], op=ALU.subtract),
             r=[ls], w=[neglam])
        K.op(DVE, lambda: dve.tensor_scalar(out=neglam.ap, in0=neglam.ap, scalar1=-LAMBDA_INIT, scalar2=None,
                                            op0=ALU.add), r=[neglam], w=[neglam])
        conv = []
        if stage >= 10:
            tf = [K.sb(es, "tf%d" % i, [128, 2048], F32) for i in range(4)]
            tb = [K.sb(es, "tb%d" % i, [128, 2048], BF16) for i in range(4)]
            n_ = 0
            for (src_, dst_) in ((uT_d, ub_d), (v_d, vbf_d)):
                for i_ in range(128):
                    def cv(f=tf[n_ % 4], bb=tb[n_ % 4], sa=src_[i_], da=dst_[i_], par=n_ % 2):
                        K.dma(SP, f.ap, sa, f.ds, w=[f])
                        if par == 0:
                            K.op(ACT, lambda: act.copy(out=bb.ap, in_=f.ap), r=[f], w=[bb])
                        else:
                            K.op(DVE, lambda: dve.tensor_copy(out=bb.ap, in_=f.ap), r=[f], w=[bb])
                        K.dma(ACT, da, bb.ap, bb.ds, r=[bb], wa=[B_tab])
                    conv.append(cv)
                    n_ += 1
        wv = wada_d.rearrange("(kc p) n -> p kc n", p=128)
        pm = psb[0]
        for blk in range(24):
            for _ in range(min(11, len(conv))):
                conv.pop(0)()
            wt = wts[blk % 2]
            K.dma(SP, wt.ap, wv[:, :, blk * 512:(blk + 1) * 512], wt.ds, w=[wt])
            for oc in range(4):
                col = (blk * 4 + oc) * NB
                for kc in range(16):
                    K.op(PE, lambda: pe.matmul(pm[:, col:col + NB], lhsT=wt[:, kc, oc * 128:(oc + 1) * 128],
                                               rhs=scT[:, kc, :], start=(kc == 0), stop=(kc == 15)),
                         r=[wt, scT], w=[pm], inc=(kc == 15))
        while conv:
            conv.pop(0)()
        K.op(DVE, lambda: dve.tensor_tensor(out=modT.ap, in0=pm[:, 0:96 * NB].rearrange("p (c b) -> p c b", b=NB),
                                            in1=bada.ap.unsqueeze(2).to_broadcast([128, 96, NB]), op=ALU.add),
             r=[pm, bada], w=[modT])
        for (GT, lo, nw) in ((G1T, 16, n1w), (G2T, 64, n2w)):
            K.op(DVE, lambda: dve.tensor_scalar(out=GT.ap, in0=modT[:, lo:lo + 16, :], scalar1=1.0, scalar2=None,
                                                op0=ALU.add), r=[modT], w=[GT])
            K.op(DVE, lambda: dve.tensor_tensor(out=GT.ap, in0=GT.ap, in1=nw.ap.unsqueeze(2).to_broadcast([128, 16, NB]),
                                                op=ALU.mult), r=[GT, nw], w=[GT])
    for i in range(NB):
        for (dst_, src_) in ((modP[i], modT), (GP1[i], G1T), (GP2[i], G2T)):
            K.op(DVE, lambda: dve.tensor_copy(out=dst_[:, :, 0:1], in_=src_[:, :, i:i + 1]), r=[src_], w=[dst_])
    K.dump("modT", modT, F32, [128, 96, NB])
    K.dump("G1T", G1T, F32, [128, 16, NB])
    K.dump("neglam", neglam, F32, [128, 1])
    K.barrier()
    SH1, G1, SH2, G2 = 0, 32, 48, 80

    def rms_stats(es_tiles, src_ap, n, ss_ap, junk_ap, rbufs, ss_t):
        K.op(ACT, lambda: act.activation(out=junk_ap, in_=src_ap, func=AF.Square, accum_out=ss_ap), r=rbufs,
             w=[ss_t] + es_tiles)
        K.op(ACT, lambda: act.activation(out=ss_ap, in_=ss_ap, func=AF.Sqrt, scale=1.0 / n, bias=epsc[:, 0:1]), r=[ss_t, epsc], w=[ss_t])
        K.op(DVE, lambda: dve.reciprocal(out=ss_ap, in_=ss_ap), r=[ss_t], w=[ss_t])

    evac_rr = [0]

    def evac_copy(out_ap, in_ap, r, w):
        evac_rr[0] += 1
        if evac_rr[0] % 2:
            return K.op(ACT, lambda: act.copy(out=out_ap, in_=in_ap), r=r, w=w)
        return K.op(DVE, lambda: dve.tensor_copy(out=out_ap, in_=in_ap), r=r, w=w)

    def load_w_block(wf, wb, w_dram, KC, c0, ncols, conv_eng):
        wv_ = w_dram.rearrange("(kc p) n -> p kc n", p=128)
        K.dma(SP, wf[:, 0:KC, 0:ncols], wv_[:, :, c0:c0 + ncols], wf.ds, w=[wf])
        if conv_eng is POOL:
            K.op(POOL, lambda: pool.tensor_copy(out=wb[:, 0:KC, 0:ncols], in_=wf[:, 0:KC, 0:ncols]), r=[wf], w=[wb])
        else:
            K.op(DVE, lambda: dve.tensor_copy(out=wb[:, 0:KC, 0:ncols], in_=wf[:, 0:KC, 0:ncols]), r=[wf], w=[wb])

    def norm_to_T(es, b, src_d, src_buf, GTsrc, shoff, hT, tagp):
        Gbc = K.sb(es, tagp + "Gbc", [128, D], F32)
        Sbc = K.sb(es, tagp + "Sbc", [128, D], F32)
        make_bc(Gbc, 0, b, GTsrc)
        make_bc(Sbc, shoff, b)
        xts = [K.sb(es, tagp + "xt%d" % i, [128, D], F32) for i in range(2)]
        xns = [K.sb(es, tagp + "xn%d" % i, [128, D], BF16) for i in range(2)]
        junk = K.sb(es, tagp + "junk", [128, D], BF16)
        sss = [K.sb(es, tagp + "ss%d" % i, [128, 1], F32) for i in range(2)]
        for tt in range(NT):
            xt, xn, ss = xts[tt % 2], xns[tt % 2], sss[tt % 2]
            K.dma(SP, xt.ap, src_d[b, tt * 128:(tt + 1) * 128, :], xt.ds, r=[src_buf], w=[xt])
            rms_stats([junk], xt.ap, D, ss.ap, junk.ap, [xt], ss)
            K.op(DVE, lambda: dve.scalar_tensor_tensor(out=xt.ap, in0=xt.ap, scalar=ss[:, 0:1], in1=Gbc.ap, op0=ALU.mult,
                                                       op1=ALU.mult), r=[xt, ss, Gbc], w=[xt])
            K.op(POOL, lambda: pool.tensor_tensor(out=xn.ap, in0=xt.ap, in1=Sbc.ap, op=ALU.add), r=[xt, Sbc], w=[xn])
            for c8 in range(2):
                pt = ps_tr[c8]
                for j in range(8):
                    ch = c8 * 8 + j
                    K.op(PE, lambda: pe.transpose(out=trv(pt)[:, j, :], in_=xn[:, ch * 128:(ch + 1) * 128],
                                                  identity=ident.ap), r=[xn, ident], w=[pt], inc=(j == 7))
                if c8 == 0:
                    K.op(ACT, lambda: act.copy(out=hT[:, 0:8, tt * 128:(tt + 1) * 128], in_=trv(pt)), r=[pt], w=[hT])
                else:
                    K.op(DVE, lambda: dve.tensor_copy(out=hT[:, 8:16, tt * 128:(tt + 1) * 128], in_=trv(pt)), r=[pt], w=[hT])

    def linear(es, aT, KC, w_dram, N, evac, tagp, conv_eng=POOL):
        wfs = [K.sb(es, tagp + "wf%d" % i, [128, KC, 512], F32) for i in range(2)]
        wbs = [K.sb(es, tagp + "wb%d" % i, [128, KC, 512], BF16) for i in range(2)]
        nblk = (N + 511) // 512
        cnt = 0
        load_w_block(wfs[0], wbs[0], w_dram, KC, 0, min(512, N), conv_eng)
        for nb in range(nblk):
            c0 = nb * 512
            ncols = min(512, N - c0)
            wf, wb = wfs[nb % 2], wbs[nb % 2]
            if nb + 1 < nblk:
                load_w_block(wfs[(nb + 1) % 2], wbs[(nb + 1) % 2], w_dram, KC, c0 + 512, min(512, N - c0 - 512), conv_eng)
            for tt in range(NT):
                ps = ps_mm[cnt % 2]
                cnt += 1
                for kc in range(KC):
                    K.op(PE, lambda: pe.matmul(ps[:, 0:ncols], lhsT=aT[:, kc, tt * 128:(tt + 1) * 128],
                                               rhs=wb[:, kc, 0:ncols], start=(kc == 0), stop=(kc == KC - 1)),
                         r=[aT, wb], w=[ps], inc=(kc == KC - 1))
                evac(ps, nb, c0, ncols, tt)

    def make_bc(dst, off, b, src=None):
        with ExitStack() as es2:
            tmp = K.sb(es2, "bctmp", [128, 16, 128], F32)
            src = modT if src is None else src
            K.op(DVE, lambda: dve.tensor_copy(out=tmp.ap, in_=src[:, off:off + 16, b:b + 1].to_broadcast([128, 16, 128])),
                 r=[src], w=[tmp])
            for q4 in range(4):
                ps = ps_mm[q4 % 2]
                for j in range(4):
                    ch = q4 * 4 + j
                    K.op(PE, lambda: pe.matmul(ps[:, j * 128:(j + 1) * 128], lhsT=tmp[:, ch, :], rhs=identf.ap,
                                               start=True, stop=True), r=[tmp, identf], w=[ps], inc=(j == 3))
                K.op(ACT, lambda: act.copy(out=dst[:, q4 * 512:(q4 + 1) * 512], in_=ps.ap), r=[ps], w=[dst])
            K.barrier()

    def rope_tables(es, b, inv, nf, tagp):
        ang = K.sb(es, tagp + "ang", [128, NT, nf], F32)
        cs = K.sb(es, tagp + "cos", [128, NT, nf], F32)
        sn = K.sb(es, tagp + "sin", [128, NT, nf], F32)
        tmp = K.sb(es, tagp + "rtmp", [128, NT, nf], F32)
        K.op(DVE, lambda: dve.tensor_tensor(out=ang.ap, in0=posf[:, b, :].unsqueeze(2).to_broadcast([128, NT, nf]),
                                            in1=inv.ap.unsqueeze(1).to_broadcast([128, NT, nf]), op=ALU.mult),
             r=[posf, inv], w=[ang])
        two_pi = 2.0 * math.pi
        ki = K.sb(es, tagp + "ki", [128, NT, nf], I32)
        kf = K.sb(es, tagp + "kf", [128, NT, nf], F32)
        for (shift, dst) in ((0.0, sn), (0.5 * math.pi, cs)):
            K.op(DVE, lambda: dve.tensor_scalar(out=tmp.ap, in0=ang.ap, scalar1=shift, scalar2=None, op0=ALU.add),
                 r=[ang], w=[tmp])
            K.op(DVE, lambda: dve.tensor_scalar(out=kf.ap, in0=tmp.ap, scalar1=1.0 / two_pi, scalar2=None, op0=ALU.mult),
                 r=[tmp], w=[kf])
            K.op(DVE, lambda: dve.tensor_copy(out=ki.ap, in_=kf.ap), r=[kf], w=[ki])
            K.op(DVE, lambda: dve.tensor_copy(out=kf.ap, in_=ki.ap), r=[ki], w=[kf])
            K.op(DVE, lambda: dve.scalar_tensor_tensor(out=tmp.ap, in0=kf.ap, scalar=-two_pi, in1=tmp.ap, op0=ALU.mult,
                                                       op1=ALU.add), r=[kf, tmp], w=[tmp])
            K.op(DVE, lambda: dve.tensor_scalar(out=kf.ap, in0=tmp.ap, scalar1=math.pi, scalar2=-two_pi, op0=ALU.is_gt,
                                                op1=ALU.mult), r=[tmp], w=[kf])
            K.op(DVE, lambda: dve.tensor_tensor(out=tmp.ap, in0=tmp.ap, in1=kf.ap, op=ALU.add), r=[tmp, kf], w=[tmp])
            K.op(DVE, lambda: dve.tensor_scalar(out=kf.ap, in0=tmp.ap, scalar1=-math.pi, scalar2=two_pi, op0=ALU.is_lt,
                                                op1=ALU.mult), r=[tmp], w=[kf])
            K.op(DVE, lambda: dve.tensor_tensor(out=tmp.ap, in0=tmp.ap, in1=kf.ap, op=ALU.add), r=[tmp, kf], w=[tmp])
            K.op(DVE, lambda: dve.tensor_scalar(out=tmp.ap, in0=tmp.ap, scalar1=3.1415925, scalar2=-3.1415925, op0=ALU.min,
                                                op1=ALU.max), r=[tmp], w=[tmp])
            K.op(ACT, lambda: act.activation(out=dst.ap, in_=tmp.ap, func=AF.Sin), r=[tmp], w=[dst])
        return cs, sn

    def hn_rope(src, H, d, gain, r0, hr, cs_ap, sn_ap, dst, T1, T2, R1, R2, SSq, rb):
        sv_ = src.ap[:, 0:H * d].rearrange("p (h d) -> p h d", h=H)
        t1 = T1.ap[:, 0:H * d].rearrange("p (h d) -> p h d", h=H)
        t2 = T2.ap[:, 0:H * d].rearrange("p (h d) -> p h d", h=H)
        dv_ = dst.ap[:, 0:H * d].rearrange("p (h d) -> p h d", h=H)
        ssq = SSq.ap[:, 0:H]
        K.op(POOL, lambda: pool.tensor_tensor(out=t1, in0=sv_, in1=sv_, op=ALU.mult), r=[src], w=[T1])
        K.op(DVE, lambda: dve.tensor_reduce(out=ssq, in_=t1, axis=AX.X, op=ALU.add), r=[T1], w=[SSq])
        K.op(ACT, lambda: act.activation(out=ssq, in_=ssq, func=AF.Sqrt, scale=1.0 / d, bias=epsc[:, 0:1]), r=[SSq, epsc], w=[SSq])
        K.op(DVE, lambda: dve.reciprocal(out=ssq, in_=ssq), r=[SSq], w=[SSq])
        K.op(DVE, lambda: dve.tensor_tensor(out=t1, in0=sv_, in1=ssq.unsqueeze(2).to_broadcast([128, H, d]), op=ALU.mult),
             r=[src, SSq], w=[T1])
        K.op(POOL, lambda: pool.tensor_tensor(out=t2, in0=t1, in1=gain.ap.unsqueeze(1).to_broadcast([128, H, d]),
                                              op=ALU.mult), r=[T1, gain], w=[T2])
        K.op(ACT, lambda: act.copy(out=dv_, in_=t2), r=[T2], w=[dst])
        x1 = t2[:, :, r0:r0 + hr]
        x2 = t2[:, :, r0 + hr:r0 + 2 * hr]
        cb = cs_ap.unsqueeze(1).to_broadcast([128, H, hr])
        sb_ = sn_ap.unsqueeze(1).to_broadcast([128, H, hr])
        ra = R1.ap[:, 0:H * hr].rearrange("p (h d) -> p h d", h=H)
        rb_ = R2.ap[:, 0:H * hr].rearrange("p (h d) -> p h d", h=H)
        K.op(DVE, lambda: dve.tensor_tensor(out=ra, in0=x1, in1=cb, op=ALU.mult), r=[T2] + rb, w=[R1])
        K.op(DVE, lambda: dve.tensor_tensor(out=rb_, in0=x2, in1=sb_, op=ALU.mult), r=[T2] + rb, w=[R2])
        K.op(DVE, lambda: dve.tensor_tensor(out=dv_[:, :, r0:r0 + hr], in0=ra, in1=rb_, op=ALU.subtract),
             r=[R1, R2, dst], w=[dst])
        K.op(DVE, lambda: dve.tensor_tensor(out=ra, in0=x1, in1=sb_, op=ALU.mult), r=[T2] + rb, w=[R1])
        K.op(DVE, lambda: dve.tensor_tensor(out=rb_, in0=x2, in1=cb, op=ALU.mult), r=[T2] + rb, w=[R2])
        K.op(DVE, lambda: dve.tensor_tensor(out=dv_[:, :, r0 + hr:r0 + 2 * hr], in0=ra, in1=rb_, op=ALU.add),
             r=[R1, R2, dst], w=[dst])

    pend = []
    opn = [0]

    def tr_piece(opc, dstT, h, qb):
        def f_():
            pt = psb[3]
            j_ = opn[0] % 8
            opn[0] += 1
            K.op(PE, lambda: pe.transpose(out=trv(pt)[:, j_, :], in_=opc.ap, identity=ident.ap), r=[opc, ident], w=[pt])
            evac_copy(dstT[:, h, qb * 128:(qb + 1) * 128], trv(pt)[:, j_, :], [pt], [dstT])
        pend.append(f_)

    def attention(es, heads, scale, finish, tagp):
        pts = [K.sb(es, tagp + "pT%d" % i, [128, 512], BF16) for i in range(4)]
        sbanks = [psb[0], psb[1], psb[2]]
        step = 0
        for hd in heads:
            hd["load"]()
            steps = []
            for qs in range(4):
                for kb in range(4 * qs + 4):
                    steps.append((qs, kb))

            def qk(i):
                qs, kb = steps[i]
                q0 = qs * 512 if kb < 4 * qs else kb * 128
                nq = (qs + 1) * 512 - q0
                ps = sbanks[i % 3]
                np_ = len(hd["parts"])
                for pi, (qt, kt, nr) in enumerate(hd["parts"]):
                    K.op(PE, lambda: pe.matmul(ps[:, 0:nq], lhsT=kt[0:nr, kb * 128:(kb + 1) * 128],
                                               rhs=qt[0:nr, q0:q0 + nq], start=(pi == 0), stop=(pi == np_ - 1)),
                         r=[qt, kt], w=[ps], inc=(pi == np_ - 1))
                return q0, nq, ps

            qq = [qk(0), qk(1)]
            for i, (qs, kb) in enumerate(steps):
                q0, nq, ps = qq.pop(0)
                if i + 2 < len(steps):
                    qq.append(qk(i + 2))
                for f_ in pend:
                    f_()
                pend.clear()
                pT = pts[step % 4]
                step += 1
                K.op(ACT, lambda: act.activation(out=pT[:, 0:nq], in_=ps[:, 0:nq], func=AF.Exp, scale=scale),
                     r=[ps], w=[pT])
                if kb >= 4 * qs:
                    K.op(POOL, lambda: pool.tensor_tensor(out=pT[:, 0:128], in0=pT[:, 0:128], in1=tri.ap, op=ALU.mult),
                         r=[pT, tri], w=[pT])
                for qb in range(q0 // 128, 4 * qs + 4):
                    acc = ps_acc[qb % 4]
                    c = qb * 128 - q0
                    K.op(PE, lambda: pe.matmul(acc[:, 0:130], lhsT=pT[:, c:c + 128], rhs=hd["v"][:, kb, :],
                                               start=(kb == 0), stop=(kb == qb)), r=[pT, hd["v"]], w=[acc])
                    if kb == qb:
                        finish(hd, qb, acc)
            for f_ in pend:
                f_()
            pend.clear()

    def transposeT(src, ncol_chunks, dstT, tt, nrows=128):
        for c8 in range((ncol_chunks + 7) // 8):
            n = min(8, ncol_chunks - c8 * 8)
            pt = ps_tr[c8 % 2]
            for j in range(n):
                ch = c8 * 8 + j
                K.op(PE, lambda: pe.transpose(out=trv(pt)[:, j, :], in_=src[:, ch * 128:(ch + 1) * 128], identity=ident.ap),
                     r=[src, ident], w=[pt], inc=(j == n - 1))
            evac_copy(dstT[:, c8 * 8:c8 * 8 + n, tt * 128:(tt + 1) * 128], trv(pt)[:, 0:n, :], [pt], [dstT])

    for b in [int(v) for v in os.environ.get("KB_LIST", "0,1").split(",")]:
        if stage < 1:
            break
        seq = ExitStack()
        with ExitStack() as es:
            hT = K.sb(es, "hT", [128, 16, S], BF16)
            with ExitStack() as es1:
                norm_to_T(es1, b, x_d, Buf("xin"), G1T, SH1, hT, "p1")
            K.barrier()
            if b == int(os.environ.get("KB_DUMP", "0")):
                K.dump("hT0", hT, BF16, [128, 16, S])
            ots = [K.sb(es, "ot%d" % i, [128, 512], F32) for i in range(3)]
            oc = [0]

            def evac_proj(ps, nb, c0, ncols, tt):
                ot = ots[oc[0] % 3]
                oc[0] += 1
                evac_copy(ot[:, 0:ncols], ps[:, 0:ncols], [ps], [ot])
                if nb == 0 and tt == 0 and b == 0:
                    K.dump("ot0", ot, F32, [128, 512])
                K.dma(STQ, proj_d[b][tt * 128:(tt + 1) * 128, c0:c0 + ncols], ot[:, 0:ncols], ot.ds, r=[ot],
                      wa=[B_proj[b]])
            linear(es, hT, 16, watt_d, ATT_IN, evac_proj, "p2")
        K.barrier()
        if stage < 3:
            seq.close()
            continue

        with ExitStack() as es:
            gq_lat = K.sb(es, "gq_lat", [128, 512], F32)
            gkv_lat = K.sb(es, "gkv_lat", [128, 512], F32)
            gq = K.sb(es, "gq", [128, 192], F32)
            gk = K.sb(es, "gk", [128, 192], F32)
            for t_, d_ in ((gq_lat, gq_lat_d), (gkv_lat, gkv_lat_d), (gq, gq_d), (gk, gk_d)):
                K.dma(SP, t_.ap, d_, t_.ds, w=[t_])
            cs, sn = rope_tables(es, b, inv_mla, 32, "m")
            wf = K.sb(es, "wf34", [128, 4, 2048], F32)
            wqb = K.sb(es, "wqb", [128, 4, 1536], BF16)
            wkvb = K.sb(es, "wkvb", [128, 4, 2048], BF16)
            K.dma(SP, wf[:, :, 0:1536], wqb_d.rearrange("(kc p) n -> p kc n", p=128), wf.ds, w=[wf])
            K.op(POOL, lambda: pool.tensor_copy(out=wqb.ap, in_=wf[:, :, 0:1536]), r=[wf], w=[wqb])
            K.dma(SP, wf.ap, wkvb_d.rearrange("(kc p) n -> p kc n", p=128), wf.ds, r=[], w=[wf])
            K.op(POOL, lambda: pool.tensor_copy(out=wkvb.ap, in_=wf.ap), r=[wf], w=[wkvb])
            lats = [K.sb(es, "lat%d" % i, [128, 1088], F32) for i in range(2)]
            latn = K.sb(es, "latn", [128, 1024], BF16)
            latT = K.sb(es, "latT", [128, 8, 128], BF16)
            junk = K.sb(es, "junk3", [128, 512], BF16)
            ss2 = K.sb(es, "ss2", [128, 2, 2], F32)
            q_sb = K.sb(es, "q_sb", [128, 1536], F32)
            kv_sb = K.sb(es, "kv_sb", [128, 2048], F32)
            k_sb = K.sb(es, "k_sb", [128, 1536], F32)
            T1 = K.sb(es, "T1", [128, 1536], F32)
            T2 = K.sb(es, "T2", [128, 1536], F32)
            R1 = K.sb(es, "R1", [128, 256], F32)
            R2 = K.sb(es, "R2", [128, 256], F32)
            SSq = K.sb(es, "SSq", [128, 8], F32)
            q_bf = K.sb(es, "q_bf", [128, 1536], BF16)
            k_bf = K.sb(es, "k_bf", [128, 1536], BF16)
            va_st = [K.sb(es, "va_st%d" % i, [128, 8, 130], BF16) for i in range(2)]
            qn_st = K.sb(es, "qn_st", [128, 8, 512], BF16)
            qr_st = K.sb(es, "qr_st", [64, 8, 512], BF16)
            kn_st = K.sb(es, "kn_st", [128, 8, 512], BF16)
            kr_st = K.sb(es, "kr_st", [64, 8, 512], BF16)
            for v_ in va_st:
                K.op(POOL, lambda: pool.memset(v_.ap, 1.0), w=[v_])
            for tt in range(NT):
                lat = lats[tt % 2]
                K.dma(SP, lat.ap, proj_d[b][tt * 128:(tt + 1) * 128, 0:1088], lat.ds, r=[B_proj[b]], w=[lat])
                for j, g_ in ((0, gq_lat), (1, gkv_lat)):
                    rms_stats([junk], lat[:, j * 512:(j + 1) * 512], 512, ss2[:, j, 0:1], junk.ap, [lat], ss2)
                    K.op(DVE, lambda: dve.scalar_tensor_tensor(out=latn[:, j * 512:(j + 1) * 512],
                                                               in0=lat[:, j * 512:(j + 1) * 512], scalar=ss2[:, j, 0:1],
                                                               in1=g_.ap, op0=ALU.mult, op1=ALU.mult),
                         r=[lat, ss2, g_], w=[latn])
                transposeT(latn, 8, latT, 0)
                cnt = 0
                for (dst, wb_, koff, nblk) in ((q_sb, wqb, 0, 3), (kv_sb, wkvb, 4, 4)):
                    for nb in range(nblk):
                        ps = ps_mm[cnt % 2]
                        cnt += 1
                        for kc in range(4):
                            K.op(PE, lambda: pe.matmul(ps.ap, lhsT=latT[:, koff + kc, :], rhs=wb_[:, kc, nb * 512:(nb + 1) * 512],
                                                       start=(kc == 0), stop=(kc == 3)), r=[latT, wb_], w=[ps], inc=(kc == 3))
                        evac_copy(dst[:, nb * 512:(nb + 1) * 512], ps.ap, [ps], [dst])
                kvv = kv_sb.ap.rearrange("p (h d) -> p h d", h=8)
                ksv = k_sb.ap.rearrange("p (h d) -> p h d", h=8)
                K.op(POOL, lambda: pool.tensor_copy(out=ksv[:, :, 0:128], in_=kvv[:, :, 0:128]), r=[kv_sb], w=[k_sb])
                K.op(POOL, lambda: pool.tensor_copy(out=ksv[:, :, 128:192],
                                                    in_=lat[:, 1024:1088].unsqueeze(1).to_broadcast([128, 8, 64])),
                     r=[lat, k_sb], w=[k_sb])
                va = va_st[tt % 2]
                K.op(POOL, lambda: pool.tensor_copy(out=va[:, :, 0:128], in_=kvv[:, :, 128:256]), r=[kv_sb], w=[va])
                K.dma(ACT, va_d[:, :, tt, :].rearrange("h p c -> p h c"), va.ap, va.ds, r=[va], wa=[B_qk])
                hn_rope(q_sb, 8, 192, gq, 128, 32, cs[:, tt, :], sn[:, tt, :], q_bf, T1, T2, R1, R2, SSq, [cs, sn])
                hn_rope(k_sb, 8, 192, gk, 128, 32, cs[:, tt, :], sn[:, tt, :], k_bf, T1, T2, R1, R2, SSq, [cs, sn])
                t4 = tt % 4
                for (src, nst, rst) in ((q_bf, qn_st, qr_st), (k_bf, kn_st, kr_st)):
                    sv3 = src.ap.rearrange("p (h d) -> p h d", h=8)
                    pt = ps_tr[0]
                    for h in range(8):
                        K.op(PE, lambda: pe.transpose(out=trv(pt)[:, h, :], in_=sv3[:, h, 0:128], identity=ident.ap),
                             r=[src, ident], w=[pt], inc=(h == 7))
                    evac_copy(nst[:, :, t4 * 128:(t4 + 1) * 128], trv(pt), [pt], [nst])
                    pt = ps_tr[1]
                    for h in range(8):
                        K.op(PE, lambda: pe.transpose(out=trv(pt)[0:64, h, :], in_=sv3[:, h, 128:192], identity=ident.ap),
                             r=[src, ident], w=[pt], inc=(h == 7))
                    evac_copy(rst[:, :, t4 * 128:(t4 + 1) * 128], trv(pt)[0:64, :, :], [pt], [rst])
                if t4 == 3:
                    t0 = (tt - 3) * 128
                    for (st_, dd, lo, hi) in ((qn_st, qT_d, 0, 128), (qr_st, qT_d, 128, 192), (kn_st, kT_d, 0, 128),
                                              (kr_st, kT_d, 128, 192)):
                        K.dma(ACT, dd[:, lo:hi, t0:t0 + 512].rearrange("h d t -> d h t"), st_.ap, st_.ds, r=[st_],
                              wa=[B_qk])
        K.barrier()
        if stage < 5:
            seq.close()
            continue

        mergedT = K.sb(seq, "mergedT", [128, 16, S], BF16)
        seq2 = ExitStack()
        seq.callback(seq2.close)
        oT = [K.sb(seq2, "oT%d" % i, [128, 8, S], BF16) for i in range(2)]
        with ExitStack() as es:
            opcs = [K.sb(es, "opc%d" % i, [128, 128], BF16) for i in range(4)]
            fcm = [0]
            hb = []
            for i in range(2):
                hb.append(dict(qn=K.sb(es, "aqn%d" % i, [128, S], BF16), qr=K.sb(es, "aqr%d" % i, [64, S], BF16),
                               kn=K.sb(es, "akn%d" % i, [128, S], BF16), kr=K.sb(es, "akr%d" % i, [64, S], BF16),
                               v=K.sb(es, "av%d" % i, [128, NT, 130], BF16)))
            recs = [K.sb(es, "rec%d" % i, [128, 1], F32) for i in range(4)]
            heads = []
            for h in range(8):
                bb = hb[h % 2]

                def load(h=h, bb=bb):
                    K.dma(SP, bb["qn"].ap, qT_d[h, 0:128, :], bb["qn"].ds, r=[B_qk], w=[bb["qn"]])
                    K.dma(SP, bb["qr"].ap, qT_d[h, 128:192, :], bb["qr"].ds, r=[B_qk], w=[bb["qr"]])
                    K.dma(SP, bb["kn"].ap, kT_d[h, 0:128, :], bb["kn"].ds, r=[B_qk], w=[bb["kn"]])
                    K.dma(SP, bb["kr"].ap, kT_d[h, 128:192, :], bb["kr"].ds, r=[B_qk], w=[bb["kr"]])
                    K.dma(SP, bb["v"].ap, va_d[h], bb["v"].ds, r=[B_qk], w=[bb["v"]])
                heads.append(dict(parts=[(bb["qn"], bb["kn"], 128), (bb["qr"], bb["kr"], 64)], v=bb["v"], h=h, load=load))

            def fin_mla(hd, qb, acc):
                rec = recs[qb % 4]
                K.op(DVE, lambda: dve.reciprocal(out=rec.ap, in_=acc[:, 128:129]), r=[acc], w=[rec])
                h = hd["h"]
                opc = opcs[fcm[0] % 4]
                fcm[0] += 1
                K.op(DVE, lambda: dve.tensor_scalar(out=opc.ap, in0=acc[:, 0:128], scalar1=rec[:, 0:1], scalar2=None, op0=ALU.mult),
                     r=[acc, rec], w=[opc])
                tr_piece(opc, oT[0], h, qb)
            attention(es, heads, 192 ** -0.5, fin_mla, "ma")
            if b == 0:
                K.dump("oaT", oT[0], BF16, [128, 8, S])
        K.barrier()
        if stage < 6:
            seq.close()
            continue

        with ExitStack() as es:
            gdq = K.sb(es, "gdq", [128, 64], F32)
            gdk = K.sb(es, "gdk", [128, 64], F32)
            K.dma(SP, gdq.ap, gdq_d, gdq.ds, w=[gdq])
            K.dma(SP, gdk.ap, gdk_d, gdk.ds, w=[gdk])
            cs, sn = rope_tables(es, b, inv_dif, 8, "d")
            dts = [K.sb(es, "dt%d" % i, [128, 3072], F32) for i in range(1)]
            T1 = K.sb(es, "dT1", [128, 1024], F32)
            T2 = K.sb(es, "dT2", [128, 1024], F32)
            R1 = K.sb(es, "dR1", [128, 128], F32)
            R2 = K.sb(es, "dR2", [128, 128], F32)
            SSq = K.sb(es, "dSSq", [128, 16], F32)
            dq_bf = K.sb(es, "dq_bf", [128, 1024], BF16)
            dk_bf = K.sb(es, "dk_bf", [128, 1024], BF16)
            vb_st = [K.sb(es, "vb_st%d" % i, [128, 8, 130], BF16) for i in range(2)]
            dq_st = K.sb(es, "dq_st", [64, 16, 256], BF16)
            dk_st = K.sb(es, "dk_st", [64, 16, 256], BF16)
            for v_ in vb_st:
                K.op(POOL, lambda: pool.memset(v_.ap, 1.0), w=[v_])
            for tt in range(NT):
                dt_ = dts[0]
                K.dma(SP, dt_.ap, proj_d[b][tt * 128:(tt + 1) * 128, 1088:4160], dt_.ds, r=[B_proj[b]], w=[dt_])
                vb = vb_st[tt % 2]
                K.op(POOL, lambda: pool.tensor_copy(out=vb[:, :, 0:128],
                                                    in_=dt_[:, 2048:3072].rearrange("p (h d) -> p h d", h=8)),
                     r=[dt_], w=[vb])
                K.dma(ACT, vb_d[:, :, tt, :].rearrange("h p c -> p h c"), vb.ap, vb.ds, r=[vb], wa=[B_dqk])
                t4 = tt % 2
                for (off, g_, dbf, dst_) in ((0, gdq, dq_bf, dq_st), (1024, gdk, dk_bf, dk_st)):
                    srcv = Tile_cols(dt_, off, 1024)
                    hn_rope(srcv, 16, 64, g_, 0, 8, cs[:, tt, :], sn[:, tt, :], dbf, T1, T2, R1, R2, SSq, [cs, sn])
                    sv3 = dbf.ap.rearrange("p (h d) -> p h d", h=16)
                    for c8 in range(2):
                        pt = ps_tr[c8]
                        for j in range(8):
                            K.op(PE, lambda: pe.transpose(out=trv(pt)[0:64, j, :], in_=sv3[:, c8 * 8 + j, :], identity=ident.ap),
                                 r=[dbf, ident], w=[pt], inc=(j == 7))
                        evac_copy(dst_[:, c8 * 8:(c8 + 1) * 8, t4 * 128:(t4 + 1) * 128], trv(pt)[0:64, :, :], [pt], [dst_])
                if t4 == 1:
                    t0 = (tt - 1) * 128
                    for (st_, dd) in ((dq_st, dqT_d), (dk_st, dkT_d)):
                        K.dma(ACT, dd[:, :, t0:t0 + 256].rearrange("h d t -> d h t"), st_.ap, st_.ds, r=[st_], wa=[B_dqk])
        K.barrier()

        with ExitStack() as es:
            opcs = [K.sb(es, "dopc%d" % i, [128, 128], BF16) for i in range(4)]
            gsub = K.sb(es, "gsub", [128, 128], F32)
            K.dma(SP, gsub.ap, gsub_d, gsub.ds, w=[gsub])
            K.op(DVE, lambda: dve.tensor_scalar(out=gsub.ap, in0=gsub.ap, scalar1=(1.0 - LAMBDA_INIT), scalar2=None,
                                                op0=ALU.mult), r=[gsub], w=[gsub])
            hb = []
            for i in range(2):
                hb.append(dict(q=K.sb(es, "dq%d" % i, [64, S], BF16), k=K.sb(es, "dk%d" % i, [64, S], BF16)))
            vts = [K.sb(es, "dv%d" % i, [128, NT, 130], BF16) for i in range(2)]
            recs = [K.sb(es, "drec%d" % i, [128, 1], F32) for i in range(4)]
            o1 = K.sb(es, "o1", [128, NT, 128], F32)
            ocs = [K.sb(es, "oc%d" % i, [128, 128], F32) for i in range(2)]
            oss = [K.sb(es, "oss%d" % i, [128, 1], F32) for i in range(2)]
            sqs = [K.sb(es, "sq7%d" % i, [128, 128], F32) for i in range(2)]
            heads = []
            for shh in range(16):
                bb = hb[shh % 2]
                vt = vts[(shh // 2) % 2]

                def load(shh=shh, bb=bb, vt=vt):
                    K.dma(SP, bb["q"].ap, dqT_d[shh], bb["q"].ds, r=[B_dqk], w=[bb["q"]])
                    K.dma(SP, bb["k"].ap, dkT_d[shh], bb["k"].ds, r=[B_dqk], w=[bb["k"]])
                    if shh % 2 == 0:
                        K.dma(SP, vt.ap, vb_d[shh // 2], vt.ds, r=[B_dqk], w=[vt])
                heads.append(dict(parts=[(bb["q"], bb["k"], 64)], v=vt, sh=shh, load=load))
            fc = [0]

            def fin_diff(hd, qb, acc):
                rec = recs[qb % 4]
                shh = hd["sh"]
                h = shh // 2
                K.op(DVE, lambda: dve.reciprocal(out=rec.ap, in_=acc[:, 128:129]), r=[acc], w=[rec])
                if shh % 2 == 0:
                    K.op(DVE, lambda: dve.tensor_scalar(out=o1[:, qb, :], in0=acc[:, 0:128], scalar1=rec[:, 0:1], scalar2=None,
                                                        op0=ALU.mult), r=[acc, rec], w=[o1])
                else:
                    oc_, os_ = ocs[fc[0] % 2], oss[fc[0] % 2]
                    sq_ = sqs[fc[0] % 2]
                    fc[0] += 1
                    K.op(DVE, lambda: dve.tensor_tensor(out=rec.ap, in0=rec.ap, in1=neglam.ap, op=ALU.mult),
                         r=[rec, neglam], w=[rec])
                    K.op(DVE, lambda: dve.scalar_tensor_tensor(out=oc_.ap, in0=acc[:, 0:128], scalar=rec[:, 0:1],
                                                               in1=o1[:, qb, :], op0=ALU.mult, op1=ALU.add),
                         r=[acc, rec, o1], w=[oc_])
                    K.op(POOL, lambda: pool.tensor_tensor(out=sq_.ap, in0=oc_.ap, in1=oc_.ap, op=ALU.mult), r=[oc_], w=[sq_])
                    K.op(DVE, lambda: dve.tensor_reduce(out=os_.ap, in_=sq_.ap, axis=AX.X, op=ALU.add), r=[sq_], w=[os_])
                    K.op(DVE, lambda: dve.tensor_scalar(out=os_.ap, in0=os_.ap, scalar1=1.0 / 128, scalar2=EPS, op0=ALU.mult,
                                                        op1=ALU.add), r=[os_], w=[os_])
                    K.op(POOL, lambda: pool.tensor_tensor(out=os_.ap, in0=os_.ap, in1=mhalf.ap, op=ALU.pow), r=[os_, mhalf], w=[os_])
                    opc = opcs[fc[0] % 4]
                    K.op(DVE, lambda: dve.scalar_tensor_tensor(out=opc.ap, in0=oc_.ap,
                                                               scalar=os_[:, 0:1], in1=gsub.ap, op0=ALU.mult, op1=ALU.mult),
                         r=[oc_, os_, gsub], w=[opc])
                    tr_piece(opc, oT[1], h, qb)
            attention(es, heads, 64 ** -0.5, fin_diff, "da")
            if b == 0:
                K.dump("obT", oT[1], BF16, [128, 8, S])
        K.barrier()
        if stage < 8:
            seq.close()
            continue

        with ExitStack() as es:
            wfs = [K.sb(es, "p8wf%d" % i, [128, 16, 512], F32) for i in range(1)]
            wbs = [K.sb(es, "p8wb%d" % i, [128, 16, 512], BF16) for i in range(1)]
            gts = [K.sb(es, "gt%d" % i, [128, 2, 512], F32) for i in range(2)]
            m1s = [K.sb(es, "m1%d" % i, [128, 512], F32) for i in range(2)]
            m2s = [K.sb(es, "m2%d" % i, [128, 512], F32) for i in range(2)]
            mbs = [K.sb(es, "mb%d" % i, [128, 512], BF16) for i in range(2)]
            n = 0
            pend8 = []
            def ld8(c0_):
                K.dma(SP, wfs[0][:, 0:8, :], wmo_d.rearrange("(kc p) n -> p kc n", p=128)[:, :, c0_:c0_ + 512], wfs[0].ds, w=[wfs[0]])
                K.dma(SP, wfs[0][:, 8:16, :], wdo_d.rearrange("(kc p) n -> p kc n", p=128)[:, :, c0_:c0_ + 512], wfs[0].ds, w=[wfs[0]])
            ld8(0)
            for nb in range(4):
                wf, wb = wfs[0], wbs[0]
                c0 = nb * 512
                K.op(POOL, lambda: pool.tensor_copy(out=wb.ap, in_=wf.ap), r=[wf], w=[wb])
                if nb + 1 < 4:
                    ld8(c0 + 512)
                for tt in range(NT):
                    gt, m1, m2, mb = gts[n % 2], m1s[n % 2], m2s[n % 2], mbs[n % 2]
                    n += 1
                    K.dma(SP, gt[:, 0, :], proj_d[b][tt * 128:(tt + 1) * 128, C_GA + c0:C_GA + c0 + 512], gt.ds,
                          r=[B_proj[b]], w=[gt])
                    K.dma(SP, gt[:, 1, :], proj_d[b][tt * 128:(tt + 1) * 128, C_GB + c0:C_GB + c0 + 512], gt.ds,
                          r=[B_proj[b]], w=[gt])
                    K.op(ACT, lambda: act.activation(out=gt.ap, in_=gt.ap, func=AF.Sigmoid), r=[gt], w=[gt])
                    pp = (ps_mm[0], ps_mm[1]) if (n % 2) else (ps_acc[0], ps_acc[1])
                    for br in range(2):
                        ps = pp[br]
                        for kc in range(8):
                            K.op(PE, lambda: pe.matmul(ps.ap, lhsT=oT[br][:, kc, tt * 128:(tt + 1) * 128],
                                                       rhs=wb[:, br * 8 + kc, :], start=(kc == 0), stop=(kc == 7)),
                                 r=[oT[br], wb], w=[ps], inc=(kc == 7))
                    while pend8:
                        pend8.pop(0)()
                    K.op(DVE, lambda: dve.tensor_tensor(out=m1.ap, in0=pp[0].ap, in1=gt[:, 0, :], op=ALU.mult),
                         r=[pp[0], gt], w=[m1])
                    K.op(DVE, lambda: dve.tensor_tensor(out=m2.ap, in0=pp[1].ap, in1=gt[:, 1, :], op=ALU.mult),
                         r=[pp[1], gt], w=[m2])
                    K.op(POOL, lambda: pool.tensor_tensor(out=mb.ap, in0=m1.ap, in1=m2.ap, op=ALU.add), r=[m1, m2], w=[mb])

                    def trm(mb=mb, nb=nb, tt=tt, pt=ps_tr[n % 2]):
                        for j in range(4):
                            K.op(PE, lambda: pe.transpose(out=trv(pt)[:, j, :], in_=mb[:, j * 128:(j + 1) * 128], identity=ident.ap),
                                 r=[mb, ident], w=[pt], inc=(j == 3))
                        K.op(ACT, lambda: act.copy(out=mergedT[:, nb * 4:nb * 4 + 4, tt * 128:(tt + 1) * 128], in_=trv(pt)[:, 0:4, :]),
                             r=[pt], w=[mergedT])
                    pend8.append(trm)
            while pend8:
                pend8.pop(0)()
        K.barrier()
        seq2.close()

        with ExitStack() as es:
            g1bc = K.sb(es, "g1bc", [128, D], F32)
            make_bc(g1bc, G1, b)
            xps = [K.sb(es, "xp%d" % i, [128, 512], F32) for i in range(3)]
            tps = [K.sb(es, "tp%d" % i, [128, 512], F32) for i in range(2)]
            n9 = [0]

            def evac_x1(ps, nb, c0, ncols, tt):
                xp, tp = xps[n9[0] % 3], tps[n9[0] % 2]
                n9[0] += 1
                K.dma(SP, xp.ap, x_d[b, tt * 128:(tt + 1) * 128, c0:c0 + 512], xp.ds, w=[xp])
                K.op(DVE, lambda: dve.tensor_tensor(out=tp.ap, in0=ps.ap, in1=g1bc[:, c0:c0 + 512], op=ALU.mult),
                     r=[ps, g1bc], w=[tp])
                K.op(POOL, lambda: pool.tensor_tensor(out=xp.ap, in0=xp.ap, in1=tp.ap, op=ALU.add), r=[xp, tp], w=[xp])
                K.dma(ACT, out_d[b, tt * 128:(tt + 1) * 128, c0:c0 + 512], xp.ap, xp.ds, r=[xp], wa=[B_out[b]])
            linear(es, mergedT, 16, wao_d, D, evac_x1, "p9")
        seq.close()
        K.barrier()
        if stage < 10:
            continue

        with ExitStack() as es:
            hT = K.sb(es, "h2T", [128, 16, S], BF16)
            with ExitStack() as es1:
                norm_to_T(es1, b, out_d, B_out[b], G2T, SH2, hT, "pa")
            K.barrier()
            K.dma(ACT, h2T_d, hT.ap, hT.ds, r=[hT], wa=[B_h2T])
            keysT = K.sb(es, "keysT", [128, 16, 128], F32)
            K.dma(SP, keysT.ap, keysT_d, keysT.ds, w=[keysT])
            wf = K.sb(es, "pawf", [128, 16, 512], F32)
            wbs = [K.sb(es, "pawb%d" % i, [128, 16, 512], BF16) for i in range(2)]
            pqs = [K.sb(es, "pq%d" % i, [128, 512], F32) for i in range(2)]
            sts = [K.sb(es, "subst%d" % i, [128, 4, 128], F32) for i in range(2)]
            n = 0
            load_w_block(wf, wbs[0], wpq_d, 16, 0, 512, POOL)
            for nb in range(4):
                wb = wbs[nb % 2]
                if nb + 1 < 4:
                    load_w_block(wf, wbs[(nb + 1) % 2], wpq_d, 16, (nb + 1) * 512, 512, POOL)
                for g in range(4):
                    gg = nb * 4 + g
                    for tb in range(4):
                        ps = ps_mm[n % 2]
                        pq = pqs[n % 2]
                        st_ = sts[n % 2]
                        n += 1
                        for kc in range(16):
                            K.op(PE, lambda: pe.matmul(ps.ap, lhsT=wb[:, kc, g * 128:(g + 1) * 128],
                                                       rhs=hT[:, kc, tb * 512:(tb + 1) * 512], start=(kc == 0), stop=(kc == 15)),
                                 r=[wb, hT], w=[ps], inc=(kc == 15))
                        K.op(ACT, lambda: act.copy(out=pq.ap, in_=ps.ap), r=[ps], w=[pq])
                        pt = ps_acc[n % 4]
                        for j in range(4):
                            K.op(PE, lambda: pe.matmul(pt[:, j * 128:(j + 1) * 128], lhsT=pq[:, j * 128:(j + 1) * 128],
                                                       rhs=keysT[:, gg, :], start=True, stop=True), r=[pq, keysT], w=[pt],
                                 inc=(j == 3))
                        K.op(DVE, lambda: dve.tensor_copy(out=st_.ap, in_=pt.ap.rearrange("p (j n) -> p j n", j=4)),
                             r=[pt], w=[st_])
                        K.dma(ACT, sub_d[tb * 512:(tb + 1) * 512, gg * 128:(gg + 1) * 128].rearrange("(j p) n -> p j n", p=128),
                              st_.ap, st_.ds, r=[st_], wa=[B_sub])
        K.barrier()

        with ExitStack() as es:
            g2bc = K.sb(es, "g2bc", [128, D], F32)
            make_bc(g2bc, G2, b)
            W_sb = K.sb(es, "W_sb", [128, 128, 256], BF16)
            h2b = K.sb(es, "h2b", [128, 16, 256], BF16)
            acc_sb = [K.sb(es, "acc_sb%d" % i, [128, D], F32) for i in range(2)]
            uts = [K.sb(es, "ut%d" % i, [128, 16, 128], BF16) for i in range(3)]
            GV = 4
            vgs = [K.sb(es, "vg%d" % i, [128, GV, D], BF16) for i in range(2)]
            a_sbs = [K.sb(es, "a_sb%d" % i, [128, 256], BF16) for i in range(2)]
            was = [K.sb(es, "wa%d" % i, [128, GV, 256], BF16) for i in range(2)]
            atmp = [K.sb(es, "atmp%d" % i, [128, 512], F32) for i in range(2)]
            subt = K.sb(es, "subt", [128, 16, 128], F32)
            subm = K.sb(es, "subm", [128, 16, 128], F32)
            sv = K.sb(es, "sv", [128, 16, 16], F32)
            si_u = K.sb(es, "si_u", [128, 16, 16], U32)
            si_f = K.sb(es, "si_f", [128, 16, 16], F32)
            cand = _TV(subm, subm.ap.rearrange("p g n -> p (g n)").rearrange("p (h c) -> p h c", h=8))
            candm = K.sb(es, "candm", [128, 8, 256], F32)
            topv = K.sb(es, "topv", [128, 8, 16], F32)
            pos_u = K.sb(es, "pos_u", [128, 8, 16], U32)
            ab_u = K.sb(es, "ab_u", [128, 2, 8, 16], U32)
            ab_f = K.sb(es, "ab_f", [128, 2, 8, 16], F32)
            eq = _TV(candm, candm.ap.rearrange("p h (a c) -> p h a c", a=16))
            ijg = K.sb(es, "ijg", [128, 3, 128], F32)
            gsum = K.sb(es, "gsum", [128, 8], F32)
            ijgTs = [[K.sb(es, "ijgT%d_%d" % (i, j), [128, 128], F32) for j in range(2)] for i in range(2)]
            GIs = [K.sb(es, "GI%d" % i, [128, 128, 8], BF16) for i in range(2)]
            OJs = [K.sb(es, "OJ%d" % i, [128, 128, 8], BF16) for i in range(2)]
            iotaT = K.sb(es, "iotaT", [128, 128, 8], BF16)
            ijbs = [[K.sb(es, "ijb%d_%d" % (i, j), [128, 2, 128], BF16) for j in range(2)] for i in range(2)]
            iota_bf = K.sb(es, "iota_bf", [128, 128], BF16)
            K.op(DVE, lambda: dve.tensor_copy(out=iota_bf.ap, in_=iota.ap), r=[iota], w=[iota_bf])
            K.op(DVE, lambda: dve.tensor_copy(out=iotaT.ap, in_=iota.ap.unsqueeze(2).to_broadcast([128, 128, 8])), r=[iota], w=[iotaT])
            xo = _TV(subt, subt.ap.rearrange("p g n -> p (g n)"))
            xo.ds = subt.ds
            ucnt = [0]
            tcnt = [0]
            NBLK = S // 256

            def make_sel(blk):
                th = []

                def T(E, meth, r, w, **kw):
                    th.append(lambda: K.op(E, lambda: getattr(E.e, meth)(**kw), r=r, w=w))
                for st in range(2):
                    r0_ = blk * 256 + st * 128
                    src_ = sub_d[r0_:r0_ + 128, :]
                    th.append(lambda src_=src_: K.dma(SP, subt.ap.rearrange("p g n -> p (g n)"), src_, subt.ds, r=[B_sub], w=[subt]))
                    for g in range(16):
                        T(DVE, "max", [subt], [sv], out=sv[:, g, 0:8], in_=subt[:, g, :])
                        T(DVE, "max_index", [subt, sv], [si_u], out=si_u[:, g, 0:8], in_max=sv[:, g, 0:8], in_values=subt[:, g, :])
                        T(DVE, "match_replace", [subt, sv], [subm], out=subm[:, g, :], in_to_replace=sv[:, g, 0:8],
                          in_values=subt[:, g, :], imm_value=NEG)
                        T(DVE, "max", [subm], [sv], out=sv[:, g, 8:16], in_=subm[:, g, :])
                        T(DVE, "max_index", [subm, sv], [si_u], out=si_u[:, g, 8:16], in_max=sv[:, g, 8:16], in_values=subm[:, g, :])
                    T(DVE, "tensor_copy", [si_u], [si_f], out=si_f.ap, in_=si_u.ap)
                    sv4 = sv.ap.rearrange("p (h two) k -> p h two k", two=2)
                    si4 = si_f.ap.rearrange("p (h two) k -> p h two k", two=2)
                    T(DVE, "tensor_tensor", [sv], [cand], out=cand.ap.rearrange("p h (a c) -> p h a c", a=16),
                      in0=sv4[:, :, 0, :].unsqueeze(3).to_broadcast([128, 8, 16, 16]),
                      in1=sv4[:, :, 1, :].unsqueeze(2).to_broadcast([128, 8, 16, 16]), op=ALU.add)
                    for h in range(8):
                        T(DVE, "max", [cand], [topv], out=topv[:, h, 0:8], in_=cand[:, h, :])
                        T(DVE, "max_index", [cand, topv], [pos_u], out=pos_u[:, h, 0:8], in_max=topv[:, h, 0:8], in_values=cand[:, h, :])
                        T(DVE, "match_replace", [cand, topv], [candm], out=candm[:, h, :], in_to_replace=topv[:, h, 0:8],
                          in_values=cand[:, h, :], imm_value=NEG)
                        T(DVE, "max", [candm], [topv], out=topv[:, h, 8:16], in_=candm[:, h, :])
                        T(DVE, "max_index", [candm, topv], [pos_u], out=pos_u[:, h, 8:16], in_max=topv[:, h, 8:16], in_values=candm[:, h, :])
                    T(DVE, "tensor_single_scalar", [pos_u], [ab_u], out=ab_u[:, 0], in_=pos_u.ap, scalar=4, op=ALU.logical_shift_right)
                    T(DVE, "tensor_single_scalar", [pos_u, ab_u], [ab_u], out=ab_u[:, 1], in_=pos_u.ap, scalar=15, op=ALU.bitwise_and)
                    T(DVE, "tensor_copy", [ab_u], [ab_f], out=ab_f.ap, in_=ab_u.ap)
                    ijv = ijg.ap.rearrange("p c (h k) -> p c h k", h=8)
                    for w_ in range(2):
                        T(DVE, "tensor_tensor", [ab_f, iota], [eq], out=eq.ap, in0=ab_f[:, w_].unsqueeze(3).to_broadcast([128, 8, 16, 16]),
                          in1=iota[:, 0:16].unsqueeze(1).unsqueeze(1).to_broadcast([128, 8, 16, 16]), op=ALU.is_equal)
                        T(DVE, "tensor_tensor", [eq, si_f], [eq], out=eq.ap, in0=eq.ap,
                          in1=si4[:, :, w_, :].unsqueeze(2).to_broadcast([128, 8, 16, 16]), op=ALU.mult)
                        T(DVE, "tensor_reduce", [eq], [ijg], out=ijv[:, w_], in_=eq.ap, axis=AX.X, op=ALU.add)
                    T(DVE, "tensor_tensor", [topv, ijg], [ijg], out=ijv[:, 2], in0=topv.ap,
                      in1=topv[:, :, 0:1].to_broadcast([128, 8, 16]), op=ALU.subtract)
                    T(ACT, "activation", [ijg], [ijg], out=ijg[:, 2, :], in_=ijg[:, 2, :], func=AF.Exp)
                    T(DVE, "tensor_reduce", [ijg], [gsum], out=gsum.ap, in_=ijv[:, 2], axis=AX.X, op=ALU.add)
                    T(DVE, "reciprocal", [gsum], [gsum], out=gsum.ap, in_=gsum.ap)
                    T(DVE, "tensor_tensor", [ijg, gsum], [ijg], out=ijv[:, 2], in0=ijv[:, 2],
                      in1=gsum.ap.unsqueeze(2).to_broadcast([128, 8, 16]), op=ALU.mult)
                    dstT = ijgTs[blk % 2][st]
                    dstB = ijbs[blk % 2][st]

                    def trs(dstT=dstT, dstB=dstB):
                        ptf = ps_tr[0]
                        for c in range(3):
                            K.op(PE, lambda: pe.transpose(out=ptf[:, c * 128:(c + 1) * 128], in_=ijg[:, c, :], identity=identf.ap),
                                 r=[ijg, identf], w=[ptf], inc=(c == 2))
                        K.op(ACT, lambda: act.copy(out=dstT.ap, in_=ptf[:, 256:384]), r=[ptf], w=[dstT])
                        K.op(ACT, lambda: act.copy(out=dstB.ap, in_=ptf[:, 0:256].rearrange("p (c t) -> p c t", c=2)),
                             r=[ptf], w=[dstB])
                    th.append(trs)
                return th

            def w_build(blk):
                cn = 0
                for st in range(2):
                    ijgT = ijgTs[blk % 2][st]
                    ijb = ijbs[blk % 2][st]
                    for hf in range(16):
                        tsl = slice(hf * 8, (hf + 1) * 8)
                        OJ_, GI_ = OJs[cn % 2], GIs[cn % 2]
                        cn += 1
                        K.op(DVE, lambda: dve.tensor_tensor(out=OJ_.ap, in0=iotaT.ap,
                                                            in1=ijb[:, 1, tsl].unsqueeze(1).to_broadcast([128, 128, 8]),
                                                            op=ALU.is_equal), r=[iotaT, ijb], w=[OJ_])
                        K.op(DVE, lambda: dve.tensor_tensor(out=GI_.ap, in0=iotaT.ap,
                                                            in1=ijb[:, 0, tsl].unsqueeze(1).to_broadcast([128, 128, 8]),
                                                            op=ALU.is_equal), r=[iotaT, ijb], w=[GI_])
                        K.op(POOL, lambda: pool.tensor_tensor(out=GI_.ap, in0=GI_.ap,
                                                              in1=ijgT[:, tsl].unsqueeze(1).to_broadcast([128, 128, 8]),
                                                              op=ALU.mult), r=[GI_, ijgT], w=[GI_])
                        for t4 in range(2):
                            pw = ps_tr[1] if (t4 % 2) else ps_tr[0]
                            for j in range(4):
                                tl = t4 * 4 + j
                                K.op(PE, lambda: pe.matmul(pw[:, j * 128:(j + 1) * 128], lhsT=OJ_[:, :, tl], rhs=GI_[:, :, tl],
                                                           start=True, stop=True), r=[OJ_, GI_], w=[pw], inc=(j == 3))
                            tg = st * 128 + hf * 8 + t4 * 4
                            K.op(ACT, lambda: act.copy(out=W_sb[:, :, tg:tg + 4], in_=pw.ap.rearrange("p (t i) -> p i t", t=4)),
                                 r=[pw], w=[W_sb])

            def a_half(grp, half):
                vg = vgs[grp % 2]
                wa = was[grp % 2]
                if half == 0:
                    K.dma(SP, vg.ap, vbf_d[grp * GV:(grp + 1) * GV].rearrange("i j d -> j i d"), vg.ds, r=[B_tab], w=[vg])
                for ii in (2 * half, 2 * half + 1):
                    i = grp * GV + ii
                    ut = uts[ucnt[0] % 3]
                    a_sb = a_sbs[ucnt[0] % 2]
                    pa = psb[0] if (ucnt[0] % 2 == 0) else psb[1]
                    pav = pa[:, 0:256]
                    ucnt[0] += 1
                    K.dma(SP, ut.ap.rearrange("p c j -> p (c j)"), ub_d[i], ut.ds, r=[B_tab], w=[ut])
                    for c in range(16):
                        K.op(PE, lambda: pe.matmul(pav, lhsT=ut[:, c, :], rhs=h2b[:, c, :], start=(c == 0), stop=(c == 15)),
                             r=[ut, h2b], w=[pa], inc=(c == 15))
                    K.op(ACT, lambda: act.activation(out=a_sb.ap, in_=pav, func=AF.Gelu), r=[pa], w=[a_sb])
                    K.op(DVE, lambda: dve.tensor_tensor(out=wa[:, ii, :], in0=a_sb.ap, in1=W_sb[:, i, :], op=ALU.mult),
                         r=[a_sb, W_sb], w=[wa])

            def v_half(grp, st):
                vg = vgs[grp % 2]
                wa = was[grp % 2]
                for ii in range(GV):
                    for n_ in range(4):
                        K.op(PE, lambda: pe.matmul(ps_acc[n_].ap, lhsT=wa[:, ii, st * 128:(st + 1) * 128],
                                                   rhs=vg[:, ii, n_ * 512:(n_ + 1) * 512], start=(ii == 0),
                                                   stop=(ii == GV - 1)), r=[wa, vg], w=[ps_acc[n_]], inc=(ii == GV - 1))
                for n_ in range(4):
                    dst = acc_sb[st][:, n_ * 512:(n_ + 1) * 512]
                    if grp == 0:
                        K.op(ACT, lambda: act.copy(out=dst, in_=ps_acc[n_].ap), r=[ps_acc[n_]], w=[acc_sb[st]])
                    else:
                        tm = atmp[tcnt[0] % 2]
                        tcnt[0] += 1
                        K.op(ACT, lambda: act.copy(out=tm.ap, in_=ps_acc[n_].ap), r=[ps_acc[n_]], w=[tm])
                        K.op(POOL, lambda: pool.tensor_tensor(out=dst, in0=dst, in1=tm.ap, op=ALU.add),
                             r=[tm, acc_sb[st]], w=[acc_sb[st]])

            ngrp = 128 // GV
            for f_ in make_sel(0):
                f_()
            w_build(0)
            for blk in range(NBLK):
                t0 = blk * 256
                if dbg and b == 0 and blk == 0:
                    pass
                K.dma(SP, h2b.ap, h2T_d[:, :, t0:t0 + 256], h2b.ds, r=[B_h2T], w=[h2b])
                th = make_sel(blk + 1) if blk + 1 < NBLK else []
                per = (len(th) + ngrp - 1) // ngrp + 1
                a_half(0, 0)
                a_half(0, 1)
                for grp in range(ngrp):
                    v_half(grp, 0)
                    if grp + 1 < ngrp:
                        a_half(grp + 1, 0)
                    v_half(grp, 1)
                    if grp + 1 < ngrp:
                        a_half(grp + 1, 1)
                    for _ in range(min(per, len(th))):
                        th.pop(0)()
                while th:
                    th.pop(0)()
                if blk + 1 < NBLK:
                    w_build(blk + 1)
                for st in range(2):
                    r0_ = t0 + st * 128
                    K.dma(SP, xo.ap, out_d[b, r0_:r0_ + 128, :], xo.ds, r=[B_out[b]], w=[xo])
                    K.op(DVE, lambda: dve.tensor_tensor(out=acc_sb[st].ap, in0=acc_sb[st].ap, in1=g2bc.ap, op=ALU.mult),
                         r=[acc_sb[st], g2bc], w=[acc_sb[st]])
                    K.op(POOL, lambda: pool.tensor_tensor(out=xo.ap, in0=xo.ap, in1=acc_sb[st].ap, op=ALU.add),
                         r=[xo, acc_sb[st]], w=[xo])
                    K.dma(ACT, out_d[b, r0_:r0_ + 128, :], xo.ap, xo.ds, r=[xo], wa=[Buf("fin")])
        K.barrier()
    K.barrier()
    top.close()
    return nc


class _TV:
    def __init__(self, parent, ap):
        self.ap = ap
        self.buf = parent.buf

    def __getitem__(self, k):
        return self.ap[k]


def Tile_view(t, tt):
    return _TV(t, t.ap[:, tt, :])


def Tile_cols(t, off, n):
    return _TV(t, t.ap[:, off:off + n])


DBG_OUT = set()
DBG = {}


def prep_shared(inp):
    f = np.float32
    sh = {}
    sh["n1wT"] = np.ascontiguousarray(inp["norm1_w"][0].reshape(16, 128).T)
    sh["n2wT"] = np.ascontiguousarray(inp["norm2_w"][0].reshape(16, 128).T)
    sh["w_ada"] = np.ascontiguousarray(inp["w_ada"][0])
    sh["b_adaT"] = np.ascontiguousarray(inp["b_ada"][0].reshape(96, 128).T)
    sh["w_att_in"] = np.ascontiguousarray(inp["w_att_in"][0])
    rep = lambda v: np.ascontiguousarray(np.broadcast_to(v[None], (128,) + v.shape))
    sh["mla_q_norm_bc"] = rep(inp["mla_q_norm"][0])
    sh["mla_kv_norm_bc"] = rep(inp["mla_kv_norm"][0])
    sh["w_mla_qb"] = np.ascontiguousarray(inp["w_mla_qb"][0])
    sh["w_mla_kvb"] = np.ascontiguousarray(inp["w_mla_kvb"][0])
    sh["mla_qk_norm_q_bc"] = rep(inp["mla_qk_norm_q"][0])
    sh["mla_qk_norm_k_bc"] = rep(inp["mla_qk_norm_k"][0])
    sh["w_mla_o"] = np.ascontiguousarray(inp["w_mla_o"][0])
    sh["diff_q_norm_bc"] = rep(inp["diff_q_norm"][0])
    sh["diff_k_norm_bc"] = rep(inp["diff_k_norm"][0])
    sh["diff_lambda_bc"] = rep(inp["diff_lambda"][0])
    sh["diff_subln_bc"] = rep(inp["diff_subln"][0])
    sh["w_diff_o"] = np.ascontiguousarray(inp["w_diff_o"][0])
    sh["w_att_out"] = np.ascontiguousarray(inp["w_att_out"][0])
    sh["w_peer_q"] = np.ascontiguousarray(inp["w_peer_q"][0])
    sh["peer_keysT"] = np.ascontiguousarray(inp["peer_keys"][0].reshape(16, 128, 128).transpose(2, 0, 1))
    u = inp["peer_u"][0].reshape(128, 128, 16, 128)
    sh["peer_uT"] = np.ascontiguousarray(u.transpose(0, 3, 2, 1)).reshape(128, 128, 2048)
    sh["peer_v"] = np.ascontiguousarray(inp["peer_v"][0].reshape(128, 128, 2048))
    sh["iota128"] = rep(np.arange(128, dtype=f))
    theta = 500000.0
    sh["inv_mla"] = rep((theta ** (-np.arange(0, 64, 2, dtype=f) / f(64))).astype(f))
    sh["inv_dif"] = rep((theta ** (-np.arange(0, 16, 2, dtype=f) / f(16))).astype(f))
    return sh


def prep_core(inp, c):
    b0 = c * NB
    m = {}
    m["x"] = np.ascontiguousarray(inp["x"][b0:b0 + NB])
    m["cT"] = np.ascontiguousarray(inp["c"][b0:b0 + NB].reshape(NB, 16, 128).transpose(2, 1, 0))
    pos = inp["positions"][b0:b0 + NB]
    m["posT"] = np.ascontiguousarray(pos.reshape(NB, NT, 128).transpose(2, 0, 1))
    return m


def kernel(**inputs):
    inp = {k: np.asarray(v) for k, v in inputs.items()}
    nc = build_nc()
    sh = prep_shared(inp)
    in_maps = []
    for c in range(8):
        m = dict(sh)
        m.update(prep_core(inp, c))
        in_maps.append(m)
    res = run_bass_kernel_spmd(nc, in_maps, core_ids=list(range(8)))
    return np.concatenate([r["out"] for r in res.results], axis=0).astype(np.float32)
```
